# Optimizing a Trainium2 kernel written in Bass

```python
import math
import jax, jax.numpy as jnp
from jax import lax
import numpy as np

D_MODEL = 1024
BATCH = 16
SEQ = 4096
DEPTH = 1

CHUNK = 64
Q_BLOCK = 128
M_HEADS = 4
M_QK_DIM = 64
M_V_DIM = 128
CONV_WIDTH = 4
A_HEADS = 4
A_HEAD_DIM = 64
A_V_DIM = 2 * A_HEAD_DIM
ROPE_THETA = 10000.0
M_WIDTH = M_HEADS * M_V_DIM
A_WIDTH = A_HEADS * A_V_DIM
MIX_WIDTH = M_WIDTH + A_WIDTH
SPLIT_SIZES = (M_HEADS * M_QK_DIM, M_HEADS * M_QK_DIM, M_WIDTH, M_WIDTH, M_HEADS, M_HEADS,
               A_HEADS * 2 * A_HEAD_DIM, A_HEADS * 2 * A_HEAD_DIM, A_WIDTH)
IN_WIDTH = sum(SPLIT_SIZES)
N_EXPERTS = 32
TOP_K = 4
D_FF = D_MODEL
SWIGLU_ALPHA = 1.702
SWIGLU_LIMIT = 7.0
EXPERT_BLOCK = 512
EPS = 1e-6

kernel_name = "hybrid_mlstm_diffattn_moe_block"


def rms_norm(t, g):
    tf = t.astype(jnp.float32)
    y = tf * lax.rsqrt(jnp.mean(tf * tf, axis=-1, keepdims=True) + EPS)
    return (y * g.astype(jnp.float32)).astype(t.dtype)


def causal_depthwise_conv(u, w, b):
    K, C = w.shape
    y = lax.conv_general_dilated(u, w[:, None, :].astype(u.dtype), window_strides=(1,),
                                 padding=[(K - 1, 0)], dimension_numbers=('NWC', 'WIO', 'NWC'),
                                 feature_group_count=C)
    return y + b.astype(u.dtype)


def rope(t, cos, sin):
    half = t.shape[-1] // 2
    t1, t2 = t[..., :half], t[..., half:]
    return jnp.concatenate([t1 * cos - t2 * sin, t2 * cos + t1 * sin], axis=-1).astype(t.dtype)


def to_chunks(t):
    B, S, H, d = t.shape
    return t.reshape(B, S // CHUNK, CHUNK, H, d).transpose(0, 3, 1, 2, 4)


def mlstm_chunkwise(q, k, v, i_pre, logf):
    L = q.shape[3]
    q, k, v = (t.astype(jnp.float32) for t in (q, k, v))
    k = k * (k.shape[-1] ** -0.5)
    b = jnp.cumsum(logf, axis=-1)
    b_last = b[..., -1]
    a = b_last[..., None] - b + i_pre
    m_loc = jnp.max(a, axis=-1)
    w_s = jnp.exp(a - m_loc[..., None])
    C_loc = jnp.einsum('bhnsv,bhnsk->bhnvk', v * w_s[..., None], k)
    n_loc = jnp.einsum('bhns,bhnsk->bhnk', w_s, k)

    def step(carry, inp):
        C, n, m = carry
        Cl, nl, ml, bl = inp
        m_new = jnp.maximum(bl + m, ml)
        decay = jnp.exp(bl + m - m_new)
        fresh = jnp.exp(ml - m_new)
        C_new = decay[..., None, None] * C + fresh[..., None, None] * Cl
        n_new = decay[..., None] * n + fresh[..., None] * nl
        return (C_new, n_new, m_new), (C, n, m)

    Bsz, H, _, _, dk = k.shape
    dv = v.shape[-1]
    init = (jnp.zeros((Bsz, H, dv, dk), jnp.float32), jnp.zeros((Bsz, H, dk), jnp.float32),
            jnp.zeros((Bsz, H), jnp.float32))
    xs = tuple(jnp.moveaxis(t, 2, 0) for t in (C_loc, n_loc, m_loc, b_last))
    _, states = lax.scan(step, init, xs)
    C_prev, n_prev, m_prev = (jnp.moveaxis(t, 0, 2) for t in states)

    causal = jnp.tril(jnp.ones((L, L), dtype=bool))
    d_log = jnp.where(causal, b[..., :, None] - b[..., None, :] + i_pre[..., None, :], -jnp.inf)
    m_inter = b + m_prev[..., None]
    m_t = jnp.maximum(m_inter, jnp.max(d_log, axis=-1))
    d_w = jnp.exp(d_log - m_t[..., None])
    inter_w = jnp.exp(m_inter - m_t)
    s = jnp.einsum('bhntk,bhnsk->bhnts', q, k) * d_w
    num = jnp.einsum('bhnts,bhnsv->bhntv', s, v) + inter_w[..., None] * jnp.einsum('bhnvk,bhntk->bhntv', C_prev, q)
    den = jnp.sum(s, axis=-1) + inter_w * jnp.einsum('bhnk,bhntk->bhnt', n_prev, q)
    return num / jnp.maximum(jnp.abs(den), jnp.exp(-m_t))[..., None]


def diff_attention(q, k, v, lam):
    B, H, _, S, dh = q.shape
    dv = v.shape[-1]
    key_chunk = jnp.arange(S) // CHUNK
    scale = dh ** -0.5

    def block(i):
        start = i * Q_BLOCK
        qb = lax.dynamic_slice_in_dim(q, start, Q_BLOCK, axis=3)
        s = jnp.einsum('bhmqd,bhmkd->bhmqk', qb, k).astype(jnp.float32) * scale
        q_chunk = (start + jnp.arange(Q_BLOCK)) // CHUNK
        mask = key_chunk[None, :] <= q_chunk[:, None]
        p = jax.nn.softmax(jnp.where(mask, s, -jnp.inf), axis=-1)
        a = p[:, :, 0] - lam * p[:, :, 1]
        return jnp.einsum('bhqk,bhkv->bhqv', a.astype(v.dtype), v)

    o = lax.map(block, jnp.arange(S // Q_BLOCK))
    return o.transpose(1, 0, 3, 2, 4).reshape(B, S, H, dv)


def moe_ffn(h, w_router, b_router, w_gu, b_gu, w_down, b_down):
    B, S, D = h.shape
    N = B * S
    hf = h.reshape(N, D)
    logits = jnp.dot(hf, w_router).astype(jnp.float32) + b_router.astype(jnp.float32)
    top_val, top_idx = lax.top_k(logits, TOP_K)
    gates = jax.nn.softmax(top_val, axis=-1)
    n_slots = N * TOP_K
    e_flat = top_idx.reshape(-1)
    tok_flat = jnp.arange(n_slots, dtype=jnp.int32) // TOP_K
    g_flat = gates.reshape(-1)
    order = jnp.argsort(e_flat)
    e_sorted = e_flat[order]
    sizes = jnp.bincount(e_flat, length=N_EXPERTS)
    group_start = jnp.cumsum(sizes) - sizes
    padded = ((sizes + EXPERT_BLOCK - 1) // EXPERT_BLOCK) * EXPERT_BLOCK
    padded_end = jnp.cumsum(padded)
    padded_start = padded_end - padded
    dest = padded_start[e_sorted] + (jnp.arange(n_slots) - group_start[e_sorted])
    n_rows = ((n_slots + EXPERT_BLOCK - 1) // EXPERT_BLOCK) * EXPERT_BLOCK + N_EXPERTS * EXPERT_BLOCK
    row_tok = jnp.zeros((n_rows,), jnp.int32).at[dest].set(tok_flat[order])
    row_gate = jnp.zeros((n_rows,), jnp.float32).at[dest].set(g_flat[order])
    n_blocks = n_rows // EXPERT_BLOCK
    block_expert = jnp.minimum(jnp.searchsorted(padded_end, jnp.arange(n_blocks) * EXPERT_BLOCK, side='right'),
                               N_EXPERTS - 1)

    def body(y, inp):
        bi, e = inp
        idx = lax.dynamic_slice_in_dim(row_tok, bi * EXPERT_BLOCK, EXPERT_BLOCK)
        g = lax.dynamic_slice_in_dim(row_gate, bi * EXPERT_BLOCK, EXPERT_BLOCK)
        xb = hf[idx]
        gu = jnp.dot(xb, w_gu[e]) + b_gu[e]
        gate = jnp.minimum(gu[:, 0::2], SWIGLU_LIMIT)
        up = jnp.clip(gu[:, 1::2], -SWIGLU_LIMIT, SWIGLU_LIMIT)
        act = (up + 1.0) * (gate * jax.nn.sigmoid(SWIGLU_ALPHA * gate))
        out = jnp.dot(act, w_down[e]) + b_down[e]
        y = y.at[idx].add(out * g[:, None].astype(out.dtype))
        return y, None

    y, _ = lax.scan(body, jnp.zeros_like(hf), (jnp.arange(n_blocks), block_expert))
    return y.reshape(B, S, D)


def setup_inputs(seed: int = 0) -> dict:
    key = jax.random.key(seed)
    ks = jax.random.split(key, 32)
    f32 = jnp.float32
    L, D = DEPTH, D_MODEL
    nrm = lambda k, shape, s: jax.random.normal(k, shape, f32) * s
    return {
        "x": nrm(ks[0], (BATCH, SEQ, D), 1.0),
        "c": nrm(ks[1], (BATCH, D), 1.0),
        "w_ada": nrm(ks[2], (L, D, 6 * D), 0.5 * D ** -0.5),
        "b_ada": nrm(ks[3], (L, 6 * D), 0.02),
        "norm1_g": 1.0 + nrm(ks[4], (L, D), 0.02),
        "w_in": nrm(ks[5], (L, D, IN_WIDTH), D ** -0.5),
        "conv_w": nrm(ks[6], (L, CONV_WIDTH, 2 * M_HEADS * M_QK_DIM), CONV_WIDTH ** -0.5),
        "conv_b": nrm(ks[7], (L, 2 * M_HEADS * M_QK_DIM), 0.02),
        "b_igate": nrm(ks[8], (L, M_HEADS), 0.1),
        "b_fgate": jnp.linspace(3.0, 6.0, M_HEADS, dtype=f32)[None, :] + nrm(ks[9], (L, M_HEADS), 0.1),
        "mlstm_norm_g": 1.0 + nrm(ks[10], (L, M_WIDTH), 0.02),
        "q_norm_g": 1.0 + nrm(ks[11], (L, A_HEAD_DIM), 0.02),
        "k_norm_g": 1.0 + nrm(ks[12], (L, A_HEAD_DIM), 0.02),
        "lambda_q1": nrm(ks[13], (L, A_HEAD_DIM), 0.1),
        "lambda_k1": nrm(ks[14], (L, A_HEAD_DIM), 0.1),
        "lambda_q2": nrm(ks[15], (L, A_HEAD_DIM), 0.1),
        "lambda_k2": nrm(ks[16], (L, A_HEAD_DIM), 0.1),
        "diff_norm_g": 1.0 + nrm(ks[17], (L, A_WIDTH), 0.02),
        "w_out": nrm(ks[18], (L, MIX_WIDTH, D), MIX_WIDTH ** -0.5),
        "norm2_g": 1.0 + nrm(ks[19], (L, D), 0.02),
        "w_router": nrm(ks[20], (L, D, N_EXPERTS), D ** -0.5),
        "b_router": nrm(ks[21], (L, N_EXPERTS), 0.01),
        "w_gu": nrm(ks[22], (L, N_EXPERTS, D, 2 * D_FF), D ** -0.5),
        "b_gu": nrm(ks[23], (L, N_EXPERTS, 2 * D_FF), 0.01),
        "w_down": nrm(ks[24], (L, N_EXPERTS, D_FF, D), D_FF ** -0.5),
        "b_down": nrm(ks[25], (L, N_EXPERTS, D), 0.01),
    }


def reference(x, c, w_ada, b_ada, norm1_g, w_in, conv_w, conv_b, b_igate, b_fgate, mlstm_norm_g,
              q_norm_g, k_norm_g, lambda_q1, lambda_k1, lambda_q2, lambda_k2, diff_norm_g, w_out,
              norm2_g, w_router, b_router, w_gu, b_gu, w_down, b_down):
    B, S, D = x.shape
    split_idx = np.cumsum(SPLIT_SIZES)[:-1].tolist()
    pos = jnp.arange(S, dtype=jnp.float32)
    inv_freq = ROPE_THETA ** (-jnp.arange(0, A_HEAD_DIM, 2, dtype=jnp.float32) / A_HEAD_DIM)
    ang = pos[:, None] * inv_freq[None, :]
    cos, sin = jnp.cos(ang).astype(x.dtype), jnp.sin(ang).astype(x.dtype)
    cond = jax.nn.silu(c)
    for l in range(DEPTH):
        lambda_init = 0.8 - 0.6 * math.exp(-0.3 * l)
        mod = jnp.dot(cond, w_ada[l]) + b_ada[l]
        shift1, scale1, gate1, shift2, scale2, gate2 = (m[:, None, :] for m in jnp.split(mod, 6, axis=-1))

        h = rms_norm(x, norm1_g[l]) * (1.0 + scale1) + shift1
        p = jnp.dot(h, w_in[l])
        mq, mk, mv, mo, mi, mf, aq, ak, av = jnp.split(p, split_idx, axis=-1)

        qk = jax.nn.silu(causal_depthwise_conv(jnp.concatenate([mq, mk], axis=-1), conv_w[l], conv_b[l]))
        mq, mk = jnp.split(qk, 2, axis=-1)
        mq = mq.reshape(B, S, M_HEADS, M_QK_DIM)
        mk = mk.reshape(B, S, M_HEADS, M_QK_DIM)
        mv = mv.reshape(B, S, M_HEADS, M_V_DIM)
        i_pre = (mi.astype(jnp.float32) + b_igate[l].astype(jnp.float32)).reshape(B, S // CHUNK, CHUNK, M_HEADS).transpose(0, 3, 1, 2)
        logf = jax.nn.log_sigmoid(mf.astype(jnp.float32) + b_fgate[l].astype(jnp.float32)).reshape(B, S // CHUNK, CHUNK, M_HEADS).transpose(0, 3, 1, 2)
        hm = mlstm_chunkwise(to_chunks(mq), to_chunks(mk), to_chunks(mv), i_pre, logf)
        hm = hm.transpose(0, 2, 3, 1, 4).reshape(B, S, M_HEADS, M_V_DIM).astype(x.dtype)
        hm = rms_norm(hm, mlstm_norm_g[l].reshape(M_HEADS, M_V_DIM))
        hm = (hm * jax.nn.sigmoid(mo).reshape(B, S, M_HEADS, M_V_DIM)).reshape(B, S, M_WIDTH)

        aq = rms_norm(aq.reshape(B, S, A_HEADS, 2, A_HEAD_DIM), q_norm_g[l]).transpose(0, 2, 3, 1, 4)
        ak = rms_norm(ak.reshape(B, S, A_HEADS, 2, A_HEAD_DIM), k_norm_g[l]).transpose(0, 2, 3, 1, 4)
        aq, ak = rope(aq, cos, sin), rope(ak, cos, sin)
        av = av.reshape(B, S, A_HEADS, A_V_DIM).transpose(0, 2, 1, 3)
        lam = (jnp.exp(jnp.sum(lambda_q1[l].astype(jnp.float32) * lambda_k1[l].astype(jnp.float32)))
               - jnp.exp(jnp.sum(lambda_q2[l].astype(jnp.float32) * lambda_k2[l].astype(jnp.float32)))
               + lambda_init)
        ha = diff_attention(aq, ak, av, lam)
        ha = (rms_norm(ha, diff_norm_g[l].reshape(A_HEADS, A_V_DIM)) * (1.0 - lambda_init)).reshape(B, S, A_WIDTH)

        mix = jnp.dot(jnp.concatenate([hm, ha], axis=-1), w_out[l])
        x = x + gate1 * mix

        h2 = rms_norm(x, norm2_g[l]) * (1.0 + scale2) + shift2
        x = x + gate2 * moe_ffn(h2, w_router[l], b_router[l], w_gu[l], b_gu[l], w_down[l], b_down[l])
    return x
```

```python
import math
from contextlib import ExitStack

import numpy as np
import concourse.bass as bass
import concourse.mybir as mybir
from concourse.bass_utils import run_bass_kernel_spmd

F32 = mybir.dt.float32
BF16 = mybir.dt.bfloat16
I32 = mybir.dt.int32
AF = mybir.ActivationFunctionType
ALU = mybir.AluOpType
AX = mybir.AxisListType

D = 1024
MQ, MK, MV, MO, MI, MF, AQ, AK, AV = 0, 256, 512, 1024, 1536, 1540, 1544, 2056, 2568
INW = 3080
NE = 32
EPS = 1e-6
LAMBDA_INIT = 0.8 - 0.6 * math.exp(0.0)
N_CORES = 8
USE_ACT_SCALE = False


class Res:
    __slots__ = ("name", "w", "r")

    def __init__(self, name):
        self.name = name
        self.w = None
        self.r = {}


class Tile:
    def __init__(self, t, name):
        self.t = t
        self.r = Res(name)
        self.subs = {}

    def __getitem__(self, k):
        return self.t[k]

    def sub(self, k):
        if k not in self.subs:
            self.subs[k] = Res("%s.%s" % (self.r.name, k))
        return self.subs[k]

    def all(self):
        return [self.r] + list(self.subs.values())


def _res(x):
    out = []
    for a in x:
        if isinstance(a, Tile):
            out.append(a.r)
        elif isinstance(a, (list, tuple)):
            out.extend(_res(a))
        else:
            out.append(a)
    return out


class Sched:
    def __init__(self, nc, es):
        self.nc = nc
        self.es = es
        self.eng = {"pe": nc.tensor, "act": nc.scalar, "dve": nc.vector, "pool": nc.gpsimd, "sp": nc.sync}
        self.sem = {k: es.enter_context(nc.semaphore("s_" + k)) for k in self.eng}
        self.cnt = {k: 0 for k in self.eng}
        self.seen = {k: {} for k in self.eng}
        self.dsem = {}
        self.free_sems = []
        self.all_dsems = []
        self.semcnt = {}
        self.ninst = 0
        self.nwait = 0

    def _wait(self, e, ev):
        sem, val, owner = ev
        if owner == e and e == "pe":
            return
        key = id(sem)
        if self.seen[e].get(key, 0) >= val:
            return
        self.eng[e].wait_ge(sem, val)
        self.seen[e][key] = val
        self.nwait += 1

    def _deps(self, e, reads, writes):
        for r in reads:
            if r.w is not None:
                self._wait(e, r.w)
        for w in writes:
            if w.w is not None and w.w[2] != e:
                self._wait(e, w.w)
            for ev in w.r.values():
                if ev[2] != e:
                    self._wait(e, ev)

    def _commit(self, ev, reads, writes):
        for r in reads:
            r.r[ev[2]] = ev
        for w in writes:
            w.w = ev
            w.r = {}

    def op(self, e, fn, reads=(), writes=()):
        reads = _res(reads)
        writes = _res(writes)
        self._deps(e, reads, writes)
        self.cnt[e] += 1
        ev = (self.sem[e], self.cnt[e], e)
        fn(self.eng[e]).then_inc(self.sem[e], 1)
        self._commit(ev, reads, writes)
        self.ninst += 1
        return ev

    def _slot(self, slot):
        if slot not in self.dsem:
            if self.free_sems:
                sem = self.free_sems.pop()
            else:
                sem = self.es.enter_context(self.nc.semaphore("d_%d" % len(self.all_dsems)))
                self.all_dsems.append(sem)
                self.semcnt[id(sem)] = 0
            self.dsem[slot] = sem

    def _auto_slot(self, reads, writes):
        for w in writes:
            if not w.name.endswith("_d"):
                return "L_" + w.name
        return "S_" + reads[0].name

    def _dma_ev(self, slot):
        sem = self.dsem[slot]
        self.semcnt[id(sem)] += 16
        return (sem, self.semcnt[id(sem)], "dma:" + slot)

    def dma(self, q, out, in_, reads=(), writes=(), slot=None, **kw):
        reads = _res(reads)
        writes = _res(writes)
        if slot is None:
            slot = self._auto_slot(reads, writes)
        self._slot(slot)
        self._deps(q, reads, writes)
        ev = self._dma_ev(slot)
        self.eng[q].dma_start(out=out, in_=in_, **kw).then_inc(ev[0], 16)
        self._commit(ev, reads, writes)
        self.ninst += 1
        return ev

    def idma(self, out, in_, idx, gather, reads=(), writes=(), slot=None, shared=False):
        reads = _res(reads)
        writes = _res(writes)
        if slot is None:
            slot = self._auto_slot(reads, writes)
        self._slot(slot)
        if shared:
            self._deps("pool", reads, [])
            for w in writes:
                if w.w is not None and w.w[2] != "dma:" + slot:
                    self._wait("pool", w.w)
                for ev in w.r.values():
                    self._wait("pool", ev)
        else:
            self._deps("pool", reads, writes)
        ev = self._dma_ev(slot)
        off = bass.IndirectOffsetOnAxis(ap=idx, axis=0)
        if gather:
            inst = self.nc.gpsimd.indirect_dma_start(out=out, out_offset=None, in_=in_, in_offset=off)
        else:
            inst = self.nc.gpsimd.indirect_dma_start(out=out, out_offset=off, in_=in_, in_offset=None)
        inst.then_inc(ev[0], 16)
        self._commit(ev, reads, writes)
        self.ninst += 1
        return ev

    def _all_events(self):
        evs = [(self.sem[k], self.cnt[k], k) for k in self.eng if self.cnt[k] > 0]
        evs += [(sem, self.semcnt[id(sem)], "dma:*") for sem in self.all_dsems if self.semcnt[id(sem)] > 0]
        return evs

    def barrier(self):
        evs = self._all_events()
        for e in self.eng:
            for ev in evs:
                if ev[2] != e:
                    self._wait(e, ev)
        self.free_sems.extend(self.dsem.values())
        self.dsem = {}

    def finish(self):
        for ev in self._all_events():
            if ev[2] != "sp":
                self._wait("sp", ev)


def interleave(gens, width):
    active = []
    it = iter(gens)
    while True:
        while len(active) < width:
            try:
                active.append(next(it))
            except StopIteration:
                break
        if not active:
            break
        for g in list(active):
            try:
                next(g)
            except StopIteration:
                active.remove(g)


class Ring:
    def __init__(self, tiles):
        self.tiles = tiles
        self.i = 0

    def next(self):
        t = self.tiles[self.i % len(self.tiles)]
        self.i += 1
        return t


class _Stop(Exception):
    pass


def build_program(NSEQ, S, BLK, stop_after=None, debug=False):
    T = NSEQ * S
    NT = T // 128
    NT5 = T // 512
    NCHS = S // 128
    NROWS = 4 * T + NE * BLK
    NBLK = NROWS // BLK
    JB = BLK // 128
    LOGB = int(math.log2(BLK))
    assert 1 << LOGB == BLK and S % 512 == 0

    nc = bass.Bass("TRN2", target_bir_lowering=False)

    def din(name, shape, dt=F32):
        return nc.dram_tensor(name, shape, dt, kind="ExternalInput").ap()

    def dscr(name, shape, dt):
        return nc.dram_tensor(name, shape, dt, kind="ExternalOutput" if debug else "Internal").ap()

    x_d = din("x", [T, D])
    c_d = din("c", [NSEQ, D])
    w_ada_d = din("w_ada_l", [128, 8, 6 * D])
    b_ada_d = din("b_ada", [1, 6 * D])
    n1g_d = din("norm1_g", [1, D])
    n2g_d = din("norm2_g", [1, D])
    w_in_d = din("w_in_l", [128, 8, INW])
    convw_d = din("conv_w", [4, 512])
    convb_d = din("conv_b", [1, 512])
    big_d = din("b_igate", [1, 4])
    bfg_d = din("b_fgate", [1, 4])
    mng_d = din("mlstm_norm_g", [1, 512])
    qng_d = din("q_norm_g", [1, 64])
    kng_d = din("k_norm_g", [1, 64])
    lq1_d = din("lambda_q1", [1, 64])
    lk1_d = din("lambda_k1", [1, 64])
    lq2_d = din("lambda_q2", [1, 64])
    lk2_d = din("lambda_k2", [1, 64])
    dng_d = din("diff_norm_g", [1, 512])
    w_out_d = din("w_out_l", [128, 8, D])
    w_rt_d = din("w_router_l", [128, 8, NE])
    b_rt_d = din("b_router", [1, NE])
    w_gu_d = din("w_gu_l", [NE * 128, 16384])
    b_gu_d = din("b_gu_l", [NE * 128, 16])
    w_dn_d = din("w_down_l", [NE * 128, 8192])
    b_dn_d = din("b_down", [NE, D])
    cos_d = din("cos_t", [S, 32])
    sin_d = din("sin_t", [S, 32])
    out_d = nc.dram_tensor("out", [T, D], F32, kind="ExternalOutput").ap()

    mod_d = dscr("mod_s", [NSEQ, 6 * D], F32)
    mqkT_d = dscr("mqkT_s", [NSEQ, 512, S], BF16)
    mk_d = dscr("mk_s", [T, 256], BF16)
    mv_d = dscr("mv_s", [T, 512], BF16)
    mo_d = dscr("mo_s", [T, 512], BF16)
    b_d = dscr("b_s", [NSEQ, 4, S], F32)
    aqT_d = dscr("aqT_s", [NSEQ, 512, S], BF16)
    akT_d = dscr("akT_s", [NSEQ, 512, S], BF16)
    av_d = dscr("av_s", [T, 512], BF16)
    cat_d = dscr("cat_s", [T, D], BF16)
    x1_d = dscr("x1_s", [T, D], F32)
    h2_d = dscr("h2_s", [T, D], BF16)
    rtok_d = dscr("rtok_s", [NROWS, 1], I32)
    oslot_d = dscr("oslot_s", [NROWS, D], F32)
    wgub_d = nc.dram_tensor("wgub_s", [NE * 128, 16384], BF16, kind="Internal").ap()
    wdnb_d = nc.dram_tensor("wdnb_s", [NE * 128, 8192], BF16, kind="Internal").ap()
    R_wgub, R_wdnb = Res("wgub_d"), Res("wdnb_d")
    R_mod, R_mqkT, R_mk, R_mv, R_mo, R_b = (Res(n) for n in ("mod_d", "mqkT_d", "mk_d", "mv_d", "mo_d", "b_d"))
    R_aqT, R_akT, R_av, R_cat, R_x1, R_h2, R_rtok, R_oslot = (
        Res(n) for n in ("aqT_d", "akT_d", "av_d", "cat_d", "x1_d", "h2_d", "rtok_d", "oslot_d"))

    with ExitStack() as es:
        Sx = Sched(nc, es)
        op = Sx.op
        usage = {}

        def mk_alloc(stack, tag):
            usage[tag] = 0

            def sb(name, shape, dt):
                n = 1
                for v in shape[1:]:
                    n *= v
                usage[tag] += n * (2 if dt == BF16 else 4)
                return Tile(stack.enter_context(nc.sbuf_tensor(tag + "_" + name, shape, dt)), tag + "_" + name)

            def ps(name, shape, dt):
                return Tile(stack.enter_context(nc.psum_tensor(tag + "_" + name, shape, dt)), tag + "_" + name)

            return sb, ps

        gsb, gps = mk_alloc(es, "g")
        identf = gsb("identf", [128, 128], F32)
        identb = gsb("identb", [128, 128], BF16)
        onesb = gsb("onesb", [128, 128], BF16)
        ustr = gsb("ustr", [128, 128], BF16)
        tmpf = gsb("tmpf", [128, 128], F32)
        op("pool", lambda e: e.memset(identf[:], 1.0), writes=[identf])
        op("pool", lambda e: e.affine_select(out=identf[:], in_=identf[:], pattern=[[-1, 128]], compare_op=ALU.is_equal,
                                             fill=0.0, base=0, channel_multiplier=1), reads=[identf], writes=[identf])
        op("dve", lambda e: e.tensor_copy(out=identb[:], in_=identf[:]), reads=[identf], writes=[identb])
        op("dve", lambda e: e.memset(onesb[:], 1.0), writes=[onesb])
        op("pool", lambda e: e.memset(tmpf[:], 1.0), writes=[tmpf])
        op("pool", lambda e: e.affine_select(out=tmpf[:], in_=tmpf[:], pattern=[[1, 128]], compare_op=ALU.is_ge,
                                             fill=0.0, base=-1, channel_multiplier=-1), reads=[tmpf], writes=[tmpf])
        op("dve", lambda e: e.tensor_copy(out=ustr[:], in_=tmpf[:]), reads=[tmpf], writes=[ustr])
        mhalf = gsb("mhalf", [128, 8], F32)
        op("pool", lambda e: e.memset(mhalf[:], -0.5), writes=[mhalf])
        A1T = gsb("A1T", [128, NSEQ, 8], F32)
        B1T = gsb("B1T", [128, NSEQ, 8], F32)
        colA = gsb("colA", [128, NT, 4], F32)
        lg_all = gsb("lg_all", [128, NT, NE], F32)
        top8_all = gsb("top8_all", [128, NT, 8], F32)
        G4 = gsb("G4", [128, NT, 4], F32)
        M_all = gsb("M_all", [128, NT, NE], BF16)
        dest_i = gsb("dest_i", [128, NT, 4], I32)
        widx = gsb("widx", [128, NBLK], I32)
        eidx = gsb("eidx", [128, NBLK], I32)

        with ExitStack() as ph:
            sb, ps = mk_alloc(ph, "p0")
            ct = sb("ct", [NSEQ, D], F32)
            sg = sb("sg", [NSEQ, D], F32)
            condT = sb("condT", [128, 8, NSEQ], F32)
            bada = sb("bada", [NSEQ, 6 * D], F32)
            modsb = sb("modsb", [NSEQ, 6 * D], F32)
            wa = Ring([sb("wa%d" % i, [128, 8, 512], F32) for i in range(2)])
            pT0 = ps("pT0", [128, 8, NSEQ], F32)
            pM = Ring([ps("pM%d" % i, [NSEQ, 512], F32) for i in range(2)])
            Sx.dma("sp", ct[:], c_d[:, :], writes=[ct])
            Sx.dma("sp", bada[:], b_ada_d[0, :].partition_broadcast(NSEQ), writes=[bada])
            op("act", lambda e: e.activation(out=sg[:], in_=ct[:], func=AF.Sigmoid), reads=[ct], writes=[sg])
            op("dve", lambda e: e.tensor_tensor(out=sg[:], in0=sg[:], in1=ct[:], op=ALU.mult), reads=[sg, ct], writes=[sg])
            for k in range(8):
                op("pe", lambda e: e.transpose(out=pT0[:, k, :], in_=sg[0:NSEQ, k * 128:(k + 1) * 128],
                                               identity=identf[0:NSEQ, 0:NSEQ]), reads=[sg, identf], writes=[pT0])
            op("dve", lambda e: e.tensor_copy(out=condT[:], in_=pT0[:]), reads=[pT0], writes=[condT])
            for cg in range(12):
                w = wa.next()
                Sx.dma("sp", w[:], w_ada_d[:, :, cg * 512:(cg + 1) * 512], writes=[w])
                pm = pM.next()
                for k in range(8):
                    op("pe", lambda e: e.matmul(pm[:], lhsT=condT[:, k, :], rhs=w[:, k, :], start=(k == 0), stop=(k == 7)),
                       reads=[condT, w], writes=[pm])
                op("dve", lambda e: e.tensor_tensor(out=modsb[:, cg * 512:(cg + 1) * 512], in0=pm[:],
                                                    in1=bada[:, cg * 512:(cg + 1) * 512], op=ALU.add),
                   reads=[pm, bada], writes=[modsb])
            Sx.dma("sp", mod_d[:, :], modsb[:], reads=[modsb], writes=[R_mod])
            sc1 = sb("sc1", [128, NSEQ, 8], F32)
            g1T = sb("g1T", [128, 8], F32)
            for s in range(NSEQ):
                Sx.dma("sp", sc1[:, s, :], mod_d[s, D:2 * D].rearrange("(k p) -> p k", p=128), reads=[R_mod], writes=[sc1], allow_slow_non_contiguous=True)
                Sx.dma("sp", B1T[:, s, :], mod_d[s, 0:D].rearrange("(k p) -> p k", p=128), reads=[R_mod], writes=[B1T], allow_slow_non_contiguous=True)
            Sx.dma("sp", g1T[:], n1g_d[0, :].rearrange("(k p) -> p k", p=128), writes=[g1T],
                   allow_slow_non_contiguous=True)
            op("dve", lambda e: e.scalar_tensor_tensor(out=A1T[:], in0=sc1[:], scalar=1.0,
                                                       in1=g1T[:].unsqueeze(1).to_broadcast([128, NSEQ, 8]),
                                                       op0=ALU.add, op1=ALU.mult), reads=[sc1, g1T], writes=[A1T])
            Sx.barrier()
        if stop_after == 0:
            Sx.finish()
            return nc

        with ExitStack() as ph:
            sb, ps = mk_alloc(ph, "p1")
            w_sb = sb("w_in", [128, 8, INW], BF16)
            for k in range(8):
                Sx.dma("pool", w_sb[:, k, :], w_in_d[:, k, :], writes=[w_sb.sub(k)])
            cw = sb("cw", [128, 4, 4], F32)
            cb = sb("cb", [128, 4], F32)
            for cch in range(4):
                Sx.dma("sp", cw[:, cch, :], convw_d[:, cch * 128:(cch + 1) * 128].rearrange("j p -> p j"), writes=[cw], allow_slow_non_contiguous=True)
            Sx.dma("sp", cb[:], convb_d[0, :].rearrange("(c p) -> p c", p=128), writes=[cb],
                   allow_slow_non_contiguous=True)
            big = sb("big", [4, 1], F32)
            bfg = sb("bfg", [4, 1], F32)
            Sx.dma("sp", big[:], big_d.rearrange("o h -> h o"), writes=[big], allow_slow_non_contiguous=True)
            Sx.dma("sp", bfg[:], bfg_d.rearrange("o h -> h o"), writes=[bfg], allow_slow_non_contiguous=True)
            gq = sb("gq", [128, 64], F32)
            gk = sb("gk", [128, 64], F32)
            Sx.dma("sp", gq[:], qng_d[0, :].partition_broadcast(128), writes=[gq])
            Sx.dma("sp", gk[:], kng_d[0, :].partition_broadcast(128), writes=[gk])
            op("dve", lambda e: e.tensor_scalar(out=gq[:], in0=gq[:], scalar1=0.125, scalar2=None, op0=ALU.mult),
               reads=[gq], writes=[gq])
            rmask = sb("rmask", [4, 512], F32)
            op("pool", lambda e: e.memset(rmask[:], 1.0), writes=[rmask])
            for j in range(4):
                op("pool", lambda e: e.memset(rmask[:, j * 128:j * 128 + 1], 0.0), reads=[rmask], writes=[rmask])

            xt = Ring([sb("xt%d" % i, [128, D], F32) for i in range(4)])
            sqj = sb("sqj", [128, D], BF16)
            ss = Ring([sb("ss%d" % i, [128, 4], F32) for i in range(2)])
            hb = Ring([sb("hb%d" % i, [128, 4, D], BF16) for i in range(1)])
            hT = Ring([sb("hT%d" % i, [128, 8, 512], BF16) for i in range(2)])
            pre = sb("pre", [128, 4, 515], F32)
            acc = Ring([sb("acc%d" % i, [128, 512], F32) for i in range(2)])
            sig = Ring([sb("sig%d" % i, [128, 512], F32) for i in range(2)])
            qkT = Ring([sb("qkT%d" % i, [128, 4, 512], BF16) for i in range(2)])
            ktok = Ring([sb("ktok%d" % i, [128, 4, 256], BF16) for i in range(2)])
            gt = {n: sb("g_" + n, [4, 512], F32) for n in ("ip", "z", "t", "b")}
            stg = {n: Ring([sb("stg_%s%d" % (n, i), [128, 512], BF16) for i in range(2)]) for n in ("mv", "mo", "av")}
            qw2 = {nm: {n: sb("qw%s_%s" % (nm, n), [128, 512], F32) for n in ("x", "xn", "t1", "t2")} for nm in ("q", "k")}
            qst2 = {nm: sb("qst" + nm, [128, 8], F32) for nm in ("q", "k")}
            qx2 = {nm: Ring([qw2[nm]["x"], sb("qwx2" + nm, [128, 512], F32)]) for nm in ("q", "k")}
            aqb = {n: Ring([sb("aqb_%s%d" % (n, i), [128, 4, 512], BF16) for i in range(1)]) for n in ("q", "k")}
            aqT = {n: Ring([sb("aqT_%s%d" % (n, i), [128, 4, 512], BF16) for i in range(1)]) for n in ("q", "k")}
            cst = Ring([sb("cst%d" % i, [128, 2, 32], F32) for i in range(3)])
            pTr = Ring([ps("pTr%d" % i, [128, 2, 512], BF16) for i in range(2)])
            pMM = Ring([ps("pMM%d" % i, [128, 512], F32) for i in range(4)])
            pMi = Ring([ps("pMi%d" % i, [128, 1024], BF16) for i in range(2)])
            op("pool", lambda e: e.memset(pre[:], 0.0), writes=[pre] + [pre.sub(c_) for c_ in range(4)])

            for i in range(NT5 if stop_after != 0.05 else 0):
                s = (i * 512) // S
                tpos = (i * 512) % S
                ssi = ss.next()
                hbi = hb.next()
                hTi = hT.next()
                xts = []
                for j in range(4):
                    xj = xt.next()
                    tb = i * 512 + j * 128
                    Sx.dma("sp", xj[:], x_d[tb:tb + 128, :], writes=[xj])
                    op("act", lambda e: e.activation(out=sqj[:], in_=xj[:], func=AF.Square, accum_out=ssi[:, j:j + 1]),
                       reads=[xj], writes=[sqj, ssi])
                    xts.append(xj)
                    if j == 3:
                        op("dve", lambda e: e.tensor_scalar(out=ssi[:], in0=ssi[:], scalar1=1.0 / D, scalar2=EPS, op0=ALU.mult,
                                                            op1=ALU.add), reads=[ssi], writes=[ssi])
                        op("pool", lambda e: e.tensor_tensor(out=ssi[:], in0=ssi[:], in1=mhalf[:, 0:4], op=ALU.pow), reads=[ssi, mhalf], writes=[ssi])
                        for jj in range(4):
                            op("dve", lambda e: e.tensor_scalar(out=hbi[:, jj, :], in0=xts[jj][:], scalar1=ssi[:, jj:jj + 1],
                                                                scalar2=None, op0=ALU.mult), reads=[xts[jj], ssi],
                               writes=[hbi.sub(jj)])
                if stop_after == 0.07:
                    continue
                for kp in range(4):
                    pt = pTr.next()
                    for kk in range(2):
                        k = kp * 2 + kk
                        for j in range(4):
                            op("pe", lambda e: e.transpose(out=pt[:, kk, j * 128:(j + 1) * 128], in_=hbi[:, j, k * 128:(k + 1) * 128],
                                                           identity=identb[:]), reads=[hbi.sub(j), identb], writes=[pt])
                    for kk in range(2):
                        k = kp * 2 + kk
                        if kk == 0 and USE_ACT_SCALE:
                            op("act", lambda e: e.activation(out=hTi[:, k, :], in_=pt[:, kk, :], func=AF.Identity,
                                                             scale=A1T[:, s, k:k + 1], bias=B1T[:, s, k:k + 1]),
                               reads=[pt, A1T, B1T], writes=[hTi.sub(k)])
                        else:
                            op("dve", lambda e: e.tensor_scalar(out=hTi[:, k, :], in0=pt[:, kk, :], scalar1=A1T[:, s, k:k + 1],
                                                                scalar2=B1T[:, s, k:k + 1], op0=ALU.mult, op1=ALU.add),
                               reads=[pt, A1T, B1T], writes=[hTi.sub(k)])
                hT_all = [hTi.sub(k) for k in range(8)]
                if stop_after == 0.1:
                    continue
                w_all = [w_sb.sub(k) for k in range(8)]
                qki = qkT.next()
                for cch in range(4):
                    pm = pMM.next()
                    for k in range(8):
                        op("pe", lambda e: e.matmul(pm[:], lhsT=w_sb[:, k, cch * 128:(cch + 1) * 128], rhs=hTi[:, k, :],
                                                    start=(k == 0), stop=(k == 7)), reads=hT_all + w_all, writes=[pm])
                    prc = pre.sub(cch)
                    if tpos == 0:
                        op("pool", lambda e: e.memset(pre[:, cch, 0:3], 0.0), writes=[prc])
                    else:
                        op("pool", lambda e: e.tensor_copy(out=pre[:, cch, 0:3], in_=pre[:, cch, 512:515]), reads=[prc], writes=[prc])
                    op("act", lambda e: e.copy(out=pre[:, cch, 3:515], in_=pm[:]), reads=[pm], writes=[prc])
                    ac = acc.next()
                    sgm = sig.next()
                    op("dve", lambda e: e.tensor_scalar(out=ac[:], in0=pre[:, cch, 3:515], scalar1=cw[:, cch, 3:4],
                                                        scalar2=cb[:, cch:cch + 1], op0=ALU.mult, op1=ALU.add),
                       reads=[prc, cw, cb], writes=[ac])
                    for tap in (2, 1, 0):
                        op("dve", lambda e: e.scalar_tensor_tensor(out=ac[:], in0=pre[:, cch, tap:tap + 512],
                                                                   scalar=cw[:, cch, tap:tap + 1], in1=ac[:],
                                                                   op0=ALU.mult, op1=ALU.add), reads=[prc, cw, ac], writes=[ac])
                    op("act", lambda e: e.activation(out=sgm[:], in_=ac[:], func=AF.Sigmoid), reads=[ac], writes=[sgm])
                    if cch < 2:
                        op("dve", lambda e: e.scalar_tensor_tensor(out=qki[:, cch, :], in0=ac[:], scalar=0.125, in1=sgm[:], op0=ALU.mult,
                                                                   op1=ALU.mult), reads=[ac, sgm], writes=[qki.sub(cch)])
                    else:
                        op("pool", lambda e: e.tensor_tensor(out=qki[:, cch, :], in0=ac[:], in1=sgm[:], op=ALU.mult),
                           reads=[ac, sgm], writes=[qki.sub(cch)])
                Sx.dma("sp", mqkT_d[s, :, tpos:tpos + 512].rearrange("(c p) t -> p c t", p=128), qki[:],
                       reads=[qki.sub(c_) for c_ in range(4)], writes=[R_mqkT])
                def k_transposes():
                    kti = ktok.next()
                    pk = pMi.next()
                    for cch in (2, 3):
                        for j in range(4):
                            op("pe", lambda e: e.transpose(out=pk[:, j * 256 + (cch - 2) * 128:j * 256 + (cch - 1) * 128],
                                                           in_=qki[:, cch, j * 128:(j + 1) * 128], identity=identb[:]),
                               reads=[qki.sub(cch), identb], writes=[pk])
                    op("act", lambda e: e.copy(out=kti[:].rearrange("p j f -> p (j f)"), in_=pk[:]), reads=[pk], writes=[kti])
                    Sx.dma("sp", mk_d[i * 512:(i + 1) * 512, :].rearrange("(j p) f -> p j f", p=128), kti[:], reads=[kti],
                           writes=[R_mk])
                if stop_after == 0.2:
                    continue
                pI = pMM.next()
                for k in range(8):
                    op("pe", lambda e: e.matmul(pI[0:4, :], lhsT=w_sb[:, k, MI:MI + 4], rhs=hTi[:, k, :], start=(k == 0), stop=(k == 7)),
                       reads=hT_all + w_all, writes=[pI])
                pF = pMM.next()
                for k in range(8):
                    op("pe", lambda e: e.matmul(pF[0:4, :], lhsT=w_sb[:, k, MF:MF + 4], rhs=hTi[:, k, :], start=(k == 0), stop=(k == 7)),
                       reads=hT_all + w_all, writes=[pF])
                g = gt
                op("dve", lambda e: e.tensor_scalar(out=g["ip"][:], in0=pI[0:4, :], scalar1=big[:, 0:1], scalar2=None, op0=ALU.add),
                   reads=[pI, big], writes=[g["ip"]])
                op("dve", lambda e: e.tensor_scalar(out=g["z"][:], in0=pF[0:4, :], scalar1=bfg[:, 0:1], scalar2=None, op0=ALU.add),
                   reads=[pF, bfg], writes=[g["z"]])
                op("dve", lambda e: e.scalar_tensor_tensor(out=g["t"][:], in0=g["z"][:], scalar=-1.0, in1=g["z"][:], op0=ALU.mult, op1=ALU.max),
                   reads=[g["z"]], writes=[g["t"]])
                op("act", lambda e: e.activation(out=g["t"][:], in_=g["t"][:], func=AF.Exp, scale=-1.0), reads=[g["t"]], writes=[g["t"]])
                op("act", lambda e: e.activation(out=g["t"][:], in_=g["t"][:], func=AF.Ln, bias=1.0), reads=[g["t"]], writes=[g["t"]])
                op("dve", lambda e: e.scalar_tensor_tensor(out=g["z"][:], in0=g["z"][:], scalar=0.0, in1=g["t"][:], op0=ALU.min,
                                                           op1=ALU.subtract), reads=[g["z"], g["t"]], writes=[g["z"]])
                op("dve", lambda e: e.tensor_tensor_scan(out=g["b"][:], data0=rmask[:], data1=g["z"][:], initial=0.0, op0=ALU.mult,
                                                         op1=ALU.add), reads=[rmask, g["z"]], writes=[g["b"]])
                Sx.dma("sp", b_d[s, :, tpos:tpos + 512], g["b"][:], reads=[g["b"]], writes=[R_b])
                op("dve", lambda e: e.tensor_tensor(out=g["ip"][:], in0=g["ip"][:], in1=g["b"][:], op=ALU.subtract),
                   reads=[g["ip"], g["b"]], writes=[g["ip"]])
                def gate_transposes():
                    pG = pMM.next()
                    for j in range(4):
                        op("pe", lambda e: e.transpose(out=pG[:, j * 4:(j + 1) * 4], in_=g["ip"][0:4, j * 128:(j + 1) * 128],
                                                       identity=identf[0:4, 0:4]), reads=[g["ip"], identf], writes=[pG])
                    op("dve", lambda e: e.tensor_copy(out=colA[:, i * 4:(i + 1) * 4, :].rearrange("p j h -> p (j h)"), in_=pG[:, 0:16]),
                       reads=[pG], writes=[colA])
                if stop_after == 0.3:
                    continue
                aqi = {n: aqb[n].next() for n in ("q", "k")}
                for j in range(4):
                    tb = i * 512 + j * 128
                    cs_ = cst.next()
                    Sx.dma("sp", cs_[:, 0, :], cos_d[tpos + j * 128:tpos + (j + 1) * 128, :], writes=[cs_])
                    Sx.dma("sp", cs_[:, 1, :], sin_d[tpos + j * 128:tpos + (j + 1) * 128, :], writes=[cs_])
                    for name, off in (("mv", MV), ("mo", MO), ("av", AV), ("q", AQ), ("k", AK)):
                        pm = pMM.next()
                        for k in range(8):
                            op("pe", lambda e: e.matmul(pm[:], lhsT=hTi[:, k, j * 128:(j + 1) * 128], rhs=w_sb[:, k, off:off + 512],
                                                        start=(k == 0), stop=(k == 7)), reads=hT_all + w_all, writes=[pm])
                        if name in ("mv", "mo", "av"):
                            st_ = stg[name].next()
                            if name == "mo":
                                op("act", lambda e: e.activation(out=st_[:], in_=pm[:], func=AF.Sigmoid), reads=[pm], writes=[st_])
                            elif name == "mv":
                                op("act", lambda e: e.copy(out=st_[:], in_=pm[:]), reads=[pm], writes=[st_])
                            else:
                                op("dve", lambda e: e.tensor_copy(out=st_[:], in_=pm[:]), reads=[pm], writes=[st_])
                            dd, rr = {"mv": (mv_d, R_mv), "mo": (mo_d, R_mo), "av": (av_d, R_av)}[name]
                            Sx.dma("sp", dd[tb:tb + 128, :], st_[:], reads=[st_], writes=[rr])
                        else:
                            gg = gq if name == "q" else gk
                            W = dict(qw2[name])
                            W["x"] = qx2[name].next()
                            W["sq"] = W["t1"]
                            qst = qst2[name]
                            op("act", lambda e: e.copy(out=W["x"][:], in_=pm[:]), reads=[pm], writes=[W["x"]])
                            op("pool", lambda e: e.tensor_tensor(out=W["sq"][:], in0=W["x"][:], in1=W["x"][:], op=ALU.mult),
                               reads=[W["x"]], writes=[W["sq"]])
                            op("dve", lambda e: e.tensor_reduce(out=qst[:], in_=W["sq"][:].rearrange("p (a d) -> p a d", d=64),
                                                                axis=AX.X, op=ALU.add), reads=[W["sq"]], writes=[qst])
                            op("dve", lambda e: e.tensor_scalar(out=qst[:], in0=qst[:], scalar1=1.0 / 64, scalar2=EPS, op0=ALU.mult,
                                                                op1=ALU.add), reads=[qst], writes=[qst])
                            op("pool", lambda e: e.tensor_tensor(out=qst[:], in0=qst[:], in1=mhalf[:, 0:8], op=ALU.pow), reads=[qst, mhalf], writes=[qst])
                            v3 = lambda t_: t_[:].rearrange("p (a d) -> p a d", d=64)
                            op("dve", lambda e: e.tensor_tensor(out=v3(W["xn"]), in0=v3(W["x"]),
                                                                in1=qst[:].unsqueeze(2).to_broadcast([128, 8, 64]), op=ALU.mult),
                               reads=[W["x"], qst], writes=[W["xn"]])
                            op("pool", lambda e: e.tensor_tensor(out=v3(W["xn"]), in0=v3(W["xn"]),
                                                                 in1=gg[:].unsqueeze(1).to_broadcast([128, 8, 64]), op=ALU.mult),
                               reads=[W["xn"], gg], writes=[W["xn"]])
                            v4 = lambda t_: t_[:].rearrange("p (a two d) -> p a two d", two=2, d=32)
                            cosb = cs_[:, 0, :].unsqueeze(1).to_broadcast([128, 8, 32])
                            sinb = cs_[:, 1, :].unsqueeze(1).to_broadcast([128, 8, 32])
                            xn4, t14, t24 = v4(W["xn"]), v4(W["t1"]), v4(W["t2"])
                            o4 = aqi[name][:, j, :].rearrange("p (a two d) -> p a two d", two=2, d=32)
                            for two in range(2):
                                op("dve", lambda e: e.tensor_tensor(out=t14[:, :, two, :], in0=xn4[:, :, two, :], in1=cosb, op=ALU.mult),
                                   reads=[W["xn"], cs_], writes=[W["t1"]])
                                op("pool", lambda e: e.tensor_tensor(out=t24[:, :, two, :], in0=xn4[:, :, 1 - two, :], in1=sinb, op=ALU.mult),
                                   reads=[W["xn"], cs_], writes=[W["t2"]])
                            op("dve", lambda e: e.tensor_tensor(out=o4[:, :, 0, :], in0=t14[:, :, 0, :], in1=t24[:, :, 0, :], op=ALU.subtract),
                               reads=[W["t1"], W["t2"]], writes=[aqi[name].sub(j)])
                            op("pool", lambda e: e.tensor_tensor(out=o4[:, :, 1, :], in0=t14[:, :, 1, :], in1=t24[:, :, 1, :], op=ALU.add),
                               reads=[W["t1"], W["t2"]], writes=[aqi[name].sub(j)])
                k_transposes()
                gate_transposes()
                for name in ("q", "k"):
                    aT = aqT[name].next()
                    for fc in range(4):
                        pa = pMi.next()
                        for j in range(4):
                            op("pe", lambda e: e.transpose(out=pa[:, j * 128:(j + 1) * 128], in_=aqi[name][:, j, fc * 128:(fc + 1) * 128],
                                                           identity=identb[:]), reads=[aqi[name].sub(j), identb], writes=[pa])
                        if fc % 2 == 0:
                            op("act", lambda e: e.copy(out=aT[:, fc, :], in_=pa[:, 0:512]), reads=[pa], writes=[aT.sub(fc)])
                        else:
                            op("dve", lambda e: e.tensor_copy(out=aT[:, fc, :], in_=pa[:, 0:512]), reads=[pa], writes=[aT.sub(fc)])
                    dd, rr = (aqT_d, R_aqT) if name == "q" else (akT_d, R_akT)
                    Sx.dma("sp", dd[s, :, tpos:tpos + 512].rearrange("(c p) t -> p c t", p=128), aT[:],
                           reads=[aT.sub(c_) for c_ in range(4)], writes=[rr])
            Sx.barrier()
        if stop_after is not None and stop_after <= 1:
            Sx.finish()
            return nc

        with ExitStack() as ph:
            sb, ps = mk_alloc(ph, "p23")
            gm = sb("gm", [128, 512], F32)
            Sx.dma("sp", gm[:], mng_d[0, :].partition_broadcast(128), writes=[gm])
            Dm = Ring([sb("Dm%d" % i, [128, 128], F32) for i in range(3)])
            ATr = Ring([sb("AT%d" % i, [128, 128], BF16) for i in range(3)])
            ebr = Ring([sb("eb%d" % i, [128, 128], F32) for i in range(3)])
            qsr = Ring([sb("qs%d" % i, [128, 128], BF16) for i in range(3)])
            dnr = Ring([sb("dn%d" % i, [128, 2], F32) for i in range(4)])
            hsq = sb("hsq", [128, 4, 128], F32)
            pSCs = [ps("pSC%d" % i, [128, 512], F32) for i in range(NSEQ)]


            def seq_gen(s):
                qTr = Ring([sb("qT%d_%d" % (s, i), [128, 2, 512], BF16) for i in range(2)])
                kTr = Ring([sb("kT%d_%d" % (s, i), [128, 2, 512], BF16) for i in range(2)])
                ktr = Ring([sb("kt%d_%d" % (s, i), [128, 256], BF16) for i in range(2)])
                v1r = Ring([sb("v1%d_%d" % (s, i), [128, 4, 129], BF16) for i in range(2)])
                bbr = Ring([sb("bb%d_%d" % (s, i), [128, 4, 128], F32) for i in range(2)])
                mor = Ring([sb("mo%d_%d" % (s, i), [128, 512], BF16) for i in range(2)])
                for t_ in v1r.tiles:
                    op("pool", lambda e: e.memset(t_[:], 1.0), writes=[t_])
                Cn = sb("Cn%d" % s, [128, 2, 129], F32)
                Cnb = sb("Cnb%d" % s, [128, 2, 129], BF16)
                wcr = Ring([sb("wc%d_%d" % (s, i), [128, 2], F32) for i in range(4)])
                Vwr = Ring([sb("Vw%d_%d" % (s, i), [128, 128], BF16) for i in range(3)])
                hraw = Ring([sb("hraw%d_%d" % (s, i), [128, 4, 128], F32) for i in range(2)])
                hst = Ring([sb("hst%d_%d" % (s, i), [128, 4], F32) for i in range(2)])
                hn = Ring([sb("hn%d_%d" % (s, i), [128, 512], F32) for i in range(1)])
                hob = Ring([sb("hob%d_%d" % (s, i), [128, 512], BF16) for i in range(2)])
                def load_grp(gi):
                    tpos = gi * 512
                    q_, k_ = qTr.next(), kTr.next()
                    Sx.dma("sp", q_[:], mqkT_d[s, 0:256, tpos:tpos + 512].rearrange("(c p) t -> p c t", p=128), reads=[R_mqkT], writes=[q_])
                    Sx.dma("sp", k_[:], mqkT_d[s, 256:512, tpos:tpos + 512].rearrange("(c p) t -> p c t", p=128), reads=[R_mqkT], writes=[k_])
                    return q_, k_

                def load_chunk(c):
                    tb = s * S + c * 128
                    kt_, v_, bb_, mo_ = ktr.next(), v1r.next(), bbr.next(), mor.next()
                    Sx.dma("sp", kt_[:], mk_d[tb:tb + 128, :], reads=[R_mk], writes=[kt_])
                    Sx.dma("sp", v_[:, :, 0:128], mv_d[tb:tb + 128, :].rearrange("p (h d) -> p h d", d=128), reads=[R_mv], writes=[v_])
                    Sx.dma("sp", bb_[:], b_d[s, :, c * 128:(c + 1) * 128].partition_broadcast(128), reads=[R_b], writes=[bb_])
                    Sx.dma("sp", mo_[:], mo_d[tb:tb + 128, :], reads=[R_mo], writes=[mo_])
                    return kt_, v_, bb_, mo_

                nxt = load_grp(0)
                nxc = load_chunk(0)
                yield
                for gi in range(S // 512):
                    i = s * (S // 512) + gi
                    q_, k_ = nxt
                    if gi + 1 < S // 512:
                        nxt = load_grp(gi + 1)
                    tpos = gi * 512
                    if tpos == 0:
                        op("pool", lambda e: e.memset(Cn[:], 0.0), writes=[Cn])
                        op("pool", lambda e: e.memset(Cnb[:], 0.0), writes=[Cnb])
                    for j in range(4):
                        ch = i * 4 + j
                        tsl = slice(j * 128, (j + 1) * 128)
                        c128 = slice(0, 128)
                        kt_, v_, bb_, mo_ = nxc
                        if gi * 4 + j + 1 < S // 128:
                            nxc = load_chunk(gi * 4 + j + 1)
                        hr = hraw.next()
                        for h in range(4):
                            base, hp = (h % 2) * 64, h // 2
                            psl = slice(base, base + 64)
                            pst = pSC = pSCs[s]
                            op("pe", lambda e: e.matmul(pst[:, 0:128], lhsT=k_[psl, hp, tsl], rhs=q_[psl, hp, tsl], start=True, stop=True),
                               reads=[k_, q_], writes=[pst])
                            yield
                            dm = Dm.next()
                            op("act", lambda e: e.activation(out=dm[:], in_=bb_[:, h, c128], func=AF.Exp, bias=colA[:, ch, h:h + 1]),
                               reads=[bb_, colA], writes=[dm])
                            op("pool", lambda e: e.affine_select(out=dm[:], in_=dm[:], pattern=[[1, 128]], compare_op=ALU.is_ge, fill=0.0,
                                                                 base=0, channel_multiplier=-1), reads=[dm], writes=[dm])
                            at = ATr.next()
                            op("dve", lambda e: e.tensor_tensor(out=at[:], in0=pst[:, 0:128], in1=dm[:], op=ALU.mult), reads=[pst, dm], writes=[at])
                            eb = ebr.next()
                            op("act", lambda e: e.activation(out=eb[:], in_=bb_[:, h, c128], func=AF.Exp), reads=[bb_], writes=[eb])
                            qs = qsr.next()
                            op("dve", lambda e: e.tensor_tensor(out=qs[psl, :], in0=q_[psl, hp, tsl], in1=eb[psl, :], op=ALU.mult),
                               reads=[q_, eb], writes=[qs])
                            if h % 2 == 0:
                                wc = wcr.next()
                                kw = Vwr.next()
                                for hh in (h, h + 1):
                                    glast = bb_[:, hh, 127:128]
                                    op("act", lambda e: e.activation(out=wc[:, hh - h:hh - h + 1], in_=colA[:, ch, hh:hh + 1], func=AF.Exp, bias=glast),
                                       reads=[colA, bb_], writes=[wc])
                                op("dve", lambda e: e.tensor_tensor(out=kw[:].rearrange("p (a d) -> p a d", d=64),
                                                                    in0=kt_[:, hp * 128:(hp + 1) * 128].rearrange("p (a d) -> p a d", d=64),
                                                                    in1=wc[:].unsqueeze(2).to_broadcast([128, 2, 64]), op=ALU.mult),
                                   reads=[kt_, wc], writes=[kw])
                            yield
                            pnd = pSC
                            op("pe", lambda e: e.matmul(pSC[:, 128:257], lhsT=at[:], rhs=v_[:, h, :], start=True, stop=False),
                               reads=[at, v_], writes=[pnd])
                            op("pe", lambda e: e.matmul(pSC[:, 128:257], lhsT=qs[psl, :], rhs=Cnb[psl, hp, :], start=False, stop=True),
                               reads=[qs, Cnb.sub(h)], writes=[pnd])
                            pcl = pSC
                            op("pe", lambda e: e.matmul(pSC[:, 257:386], lhsT=kw[:], rhs=v_[:, h, :], start=True, stop=True),
                               reads=[kw, v_], writes=[pcl])
                            yield
                            op("dve", lambda e: e.scalar_tensor_tensor(out=Cn[psl, hp, :], in0=Cn[psl, hp, :], scalar=eb[psl, 127:128],
                                                                       in1=pSC[psl, 257:386], op0=ALU.mult, op1=ALU.add),
                               reads=[Cn.sub(h), eb, pcl], writes=[Cn.sub(h)])
                            op("act", lambda e: e.copy(out=Cnb[psl, hp, :], in_=Cn[psl, hp, :]), reads=[Cn.sub(h)], writes=[Cnb.sub(h)])
                            dn = dnr.next()
                            op("dve", lambda e: e.tensor_scalar(out=dn[:, 1:2], in0=pSC[:, 256:257], scalar1=-1.0, scalar2=None, op0=ALU.mult),
                               reads=[pnd], writes=[dn])
                            op("dve", lambda e: e.scalar_tensor_tensor(out=dn[:, 0:1], in0=pSC[:, 256:257], scalar=1.0, in1=dn[:, 1:2], op0=ALU.max, op1=ALU.max),
                               reads=[pnd, dn], writes=[dn])
                            op("dve", lambda e: e.reciprocal(out=dn[:, 1:2], in_=dn[:, 0:1]), reads=[dn], writes=[dn])
                            op("dve", lambda e: e.tensor_scalar(out=hr[:, h, :], in0=pSC[:, 128:256], scalar1=dn[:, 1:2], scalar2=None, op0=ALU.mult),
                               reads=[pnd, dn], writes=[hr.sub(h)])
                            yield
                        hr_all = [hr.sub(h) for h in range(4)]
                        st_ = hst.next()
                        op("pool", lambda e: e.tensor_tensor(out=hsq[:], in0=hr[:], in1=hr[:], op=ALU.mult), reads=hr_all, writes=[hsq])
                        op("dve", lambda e: e.tensor_reduce(out=st_[:], in_=hsq[:], axis=AX.X, op=ALU.add), reads=[hsq], writes=[st_])
                        op("dve", lambda e: e.tensor_scalar(out=st_[:], in0=st_[:], scalar1=1.0 / 128, scalar2=EPS, op0=ALU.mult, op1=ALU.add),
                           reads=[st_], writes=[st_])
                        op("pool", lambda e: e.tensor_tensor(out=st_[:], in0=st_[:], in1=mhalf[:, 0:4], op=ALU.pow), reads=[st_, mhalf], writes=[st_])
                        hn_ = hn.next()
                        ho_ = hob.next()
                        op("dve", lambda e: e.tensor_tensor(out=hn_[:].rearrange("p (h d) -> p h d", d=128), in0=hr[:],
                                                            in1=st_[:].unsqueeze(2).to_broadcast([128, 4, 128]), op=ALU.mult),
                           reads=hr_all + [st_], writes=[hn_])
                        op("pool", lambda e: e.tensor_tensor(out=hn_[:], in0=hn_[:], in1=gm[:], op=ALU.mult), reads=[hn_, gm], writes=[hn_])
                        op("pool", lambda e: e.tensor_tensor(out=ho_[:], in0=hn_[:], in1=mo_[:], op=ALU.mult), reads=[hn_, mo_], writes=[ho_])
                        tb = i * 512 + j * 128
                        Sx.dma("sp", cat_d[tb:tb + 128, 0:512], ho_[:], reads=[ho_], writes=[R_cat])
                        yield

            NB = S // 128
            NQT = S // 512
            lam4 = sb("lam4", [128, 4, 64], F32)
            lamj = sb("lamj", [128, 64], F32)
            lams = sb("lams", [128, 4], F32)
            for n_, d_ in enumerate((lq1_d, lk1_d, lq2_d, lk2_d)):
                Sx.dma("sp", lam4[:, n_, :], d_[0, :].partition_broadcast(128), writes=[lam4])
            for n_ in range(2):
                op("dve", lambda e: e.tensor_tensor(out=lamj[:], in0=lam4[:, 2 * n_, :], in1=lam4[:, 2 * n_ + 1, :], op=ALU.mult),
                   reads=[lam4], writes=[lamj])
                op("dve", lambda e: e.tensor_reduce(out=lams[:, n_:n_ + 1], in_=lamj[:], axis=AX.X, op=ALU.add), reads=[lamj], writes=[lams])
            op("act", lambda e: e.activation(out=lams[:, 0:2], in_=lams[:, 0:2], func=AF.Exp), reads=[lams], writes=[lams])
            op("dve", lambda e: e.tensor_tensor(out=lams[:, 2:3], in0=lams[:, 1:2], in1=lams[:, 0:1], op=ALU.subtract), reads=[lams], writes=[lams])
            op("dve", lambda e: e.tensor_scalar(out=lams[:, 2:3], in0=lams[:, 2:3], scalar1=-LAMBDA_INIT, scalar2=None, op0=ALU.add),
               reads=[lams], writes=[lams])
            dg = sb("dg", [128, 512], F32)
            Sx.dma("sp", dg[:], dng_d[0, :].partition_broadcast(128), writes=[dg])
            op("dve", lambda e: e.tensor_scalar(out=dg[:], in0=dg[:], scalar1=1.0 - LAMBDA_INIT, scalar2=None, op0=ALU.mult),
               reads=[dg], writes=[dg])
            qTa = Ring([sb("qTa%d" % i, [128, S], BF16) for i in range(2)])
            kTa = Ring([sb("kTa%d" % i, [128, S], BF16) for i in range(2)])
            v1a = Ring([sb("v1a%d" % i, [128, NB, 129], BF16) for i in range(2)])
            for t_ in v1a.tiles:
                op("pool", lambda e: e.memset(t_[:], 1.0), writes=[t_])
            Pr = Ring([sb("P%d" % i, [128, 512], BF16) for i in range(6)])
            rcr = Ring([sb("rc%d" % i, [128, 4], F32) for i in range(8)])
            t1r = Ring([sb("t1_%d" % i, [128, 128], F32) for i in range(4)])
            o_r = Ring([sb("o_%d" % i, [128, 128], F32) for i in range(8)])
            osqr = Ring([sb("osq%d" % i, [128, 128], F32) for i in range(4)])
            har = Ring([sb("ha%d" % i, [128, 4, 128], BF16) for i in range(2)])
            accS = Ring([[[sb("accS%d_%d%d" % (i, m, gp), [128, 2, 129], F32) for gp in range(2)] for m in range(2)] for i in range(2)])
            pS = Ring([ps("pS%d" % i, [128, 512], F32) for i in range(2)])
            pAcc = [[ps("pA%d%d" % (m, gp), [128, 2, 129], F32) for gp in range(2)] for m in range(2)]

            wstg = Ring([sb("wstg%d" % i, [128, 4096], BF16) for i in range(4)])
            chunks = []
            for e_ in range(NE):
                rs = slice(e_ * 128, (e_ + 1) * 128)
                for c0_ in range(0, 16384, 4096):
                    chunks.append((w_gu_d[rs, c0_:c0_ + 4096], wgub_d[rs, c0_:c0_ + 4096]))
                for c0_ in range(0, 8192, 4096):
                    chunks.append((w_dn_d[rs, c0_:c0_ + 4096], wdnb_d[rs, c0_:c0_ + 4096]))
            pc_state = {"ld": 0, "st": 0, "tiles": [], "tick": 0}
            PC_LAG = 2

            def precast_store():
                c_ = pc_state["st"]
                Sx.dma("sp", chunks[c_][1], pc_state["tiles"][c_][:], reads=[pc_state["tiles"][c_]], writes=[])
                pc_state["st"] += 1

            def precast_tick():
                if pc_state["st"] < pc_state["ld"] and pc_state["ld"] - pc_state["st"] >= PC_LAG:
                    precast_store()
                if pc_state["ld"] < len(chunks):
                    c_ = pc_state["ld"]
                    t_ = wstg.next()
                    Sx.dma("pool", t_[:], chunks[c_][0], writes=[t_])
                    pc_state["tiles"].append(t_)
                    pc_state["ld"] += 1
                elif pc_state["st"] < pc_state["ld"]:
                    precast_store()

            def precast_to(n_ld):
                while pc_state["ld"] < min(n_ld, len(chunks)) or (n_ld >= len(chunks) and pc_state["st"] < len(chunks)):
                    precast_tick()

            def load_head(sh):
                s, h = divmod(sh, 4)
                q_, k_, v_ = qTa.next(), kTa.next(), v1a.next()
                z = sh % 2
                Sx.dma("sp", q_[:], aqT_d[s, h * 128:(h + 1) * 128, :], reads=[R_aqT], writes=[q_])
                Sx.dma("sp", k_[:], akT_d[s, h * 128:(h + 1) * 128, :], reads=[R_akT], writes=[k_])
                Sx.dma("sp", v_[:, :, 0:128], av_d[s * S:(s + 1) * S, h * 128:(h + 1) * 128].rearrange("(kb p) d -> p kb d", p=128),
                       reads=[R_av], writes=[v_])
                return q_, k_, v_

            n_attn_steps = NSEQ * 4 * sum(2 * (4 * jq_ + 4) for jq_ in range(S // 512))

            def attn_gen():
                fin_state = {"f": None}
                nxt = load_head(0)
                for sh in range(NSEQ * 4):
                    s, h = divmod(sh, 4)
                    q_, k_, v_ = nxt
                    if sh + 1 < NSEQ * 4:
                        nxt = load_head(sh + 1)
                    for jq in range(NQT):
                        it_ = sh * NQT + jq
                        qend = (jq + 1) * 512
                        nkb = 4 * jq + 4
                        steps = []
                        for kb in range(nkb):
                            qlo = max(jq * 512, kb * 128)
                            for m in range(2):
                                steps.append((kb, m, qlo, qend - qlo, (qlo - jq * 512) // 128))

                        def issue_st(st):
                            kb, m, qlo, ncols, g0 = st
                            msl = slice(m * 64, m * 64 + 64)
                            pst = pS.next()
                            op("pe", lambda e: e.matmul(pst[:, 0:ncols], lhsT=k_[msl, kb * 128:(kb + 1) * 128], rhs=q_[msl, qlo:qend],
                                                        start=True, stop=True), reads=[k_, q_], writes=[pst])
                            P = Pr.next()
                            op("act", lambda e: e.activation(out=P[:, 0:ncols], in_=pst[:, 0:ncols], func=AF.Exp), reads=[pst], writes=[P])
                            if kb >= 4 * jq:
                                op("dve", lambda e: e.memset(P[64:128, 0:64], 0.0), reads=[P], writes=[P])
                            return P

                        def issue_pv(st, P):
                            kb, m, qlo, ncols, g0 = st
                            for g in range(g0, 4):
                                c0 = (g - g0) * 128
                                pa = pAcc[m][g // 2]
                                op("pe", lambda e: e.matmul(pa[:, g % 2, :], lhsT=P[:, c0:c0 + 128], rhs=v_[:, kb, :],
                                                            start=(kb == 0 and g % 2 == 0), stop=(kb == 4 * jq + g),
                                                            skip_group_check=True), reads=[P, v_], writes=[pa])

                        pend_ = []
                        for si_, st in enumerate(steps):
                            pend_.append((st, issue_st(st)))
                            if si_ == 2 and fin_state["f"] is not None:
                                yield from fin_state["f"]
                                fin_state["f"] = None
                            if len(pend_) > 2:
                                issue_pv(*pend_.pop(0))
                            pc_state["tick"] += 1
                            if pc_state["tick"] * len(chunks) // n_attn_steps > pc_state["ld"]:
                                precast_tick()
                            yield
                        if fin_state["f"] is not None:
                            yield from fin_state["f"]
                            fin_state["f"] = None
                        while pend_:
                            issue_pv(*pend_.pop(0))

                        def finalize(s=s, h=h, jq=jq):
                            ha_ = har.next()
                            acs = accS.next()
                            for m_ in range(2):
                                for gp_ in range(2):
                                    op("dve", lambda e: e.tensor_copy(out=acs[m_][gp_][:], in_=pAcc[m_][gp_][:]), reads=[pAcc[m_][gp_]], writes=[acs[m_][gp_]])
                            yield
                            rcs, os_, sqs = [], [], []
                            for g in range(4):
                                a0 = acs[0][g // 2]
                                a1 = acs[1][g // 2]
                                rc = rcr.next()
                                op("dve", lambda e: e.reciprocal(out=rc[:, 0:1], in_=a0[:, g % 2, 128:129]), reads=[a0], writes=[rc])
                                op("dve", lambda e: e.reciprocal(out=rc[:, 1:2], in_=a1[:, g % 2, 128:129]), reads=[a1], writes=[rc])
                                op("dve", lambda e: e.tensor_tensor(out=rc[:, 1:2], in0=rc[:, 1:2], in1=lams[:, 2:3], op=ALU.mult), reads=[rc, lams], writes=[rc])
                                t1 = t1r.next()
                                o_ = o_r.next()
                                op("dve", lambda e: e.tensor_scalar(out=t1[:], in0=a1[:, g % 2, 0:128], scalar1=rc[:, 1:2], scalar2=None, op0=ALU.mult),
                                   reads=[a1, rc], writes=[t1])
                                op("dve", lambda e: e.scalar_tensor_tensor(out=o_[:], in0=a0[:, g % 2, 0:128], scalar=rc[:, 0:1], in1=t1[:],
                                                                           op0=ALU.mult, op1=ALU.add), reads=[a0, rc, t1], writes=[o_])
                                rcs.append(rc)
                                os_.append(o_)
                            yield
                            for g in range(4):
                                sq_ = osqr.next()
                                op("pool", lambda e: e.tensor_tensor(out=sq_[:], in0=os_[g][:], in1=os_[g][:], op=ALU.mult), reads=[os_[g]], writes=[sq_])
                                sqs.append(sq_)
                            yield
                            for g in range(4):
                                rc = rcs[g]
                                op("dve", lambda e: e.tensor_reduce(out=rc[:, 2:3], in_=sqs[g][:], axis=AX.X, op=ALU.add), reads=[sqs[g]], writes=[rc])
                                op("dve", lambda e: e.tensor_scalar(out=rc[:, 2:3], in0=rc[:, 2:3], scalar1=1.0 / 128, scalar2=EPS, op0=ALU.mult, op1=ALU.add),
                                   reads=[rc], writes=[rc])
                            yield
                            for g in range(4):
                                rc = rcs[g]
                                op("pool", lambda e: e.tensor_tensor(out=rc[:, 3:4], in0=rc[:, 2:3], in1=mhalf[:, 0:1], op=ALU.pow), reads=[rc, mhalf], writes=[rc])
                            yield
                            for g in range(4):
                                rc = rcs[g]
                                op("dve", lambda e: e.scalar_tensor_tensor(out=ha_[:, g, :], in0=os_[g][:], scalar=rc[:, 3:4], in1=dg[:, h * 128:(h + 1) * 128],
                                                                           op0=ALU.mult, op1=ALU.mult), reads=[os_[g], rc, dg], writes=[ha_.sub(g)])
                            t0 = s * S + jq * 512
                            Sx.dma("sp", cat_d[t0:t0 + 512, 512 + h * 128:512 + (h + 1) * 128].rearrange("(g p) d -> p g d", p=128), ha_[:],
                                   reads=[ha_.sub(g) for g in range(4)], writes=[R_cat])

                        fin_state["f"] = finalize()
                if fin_state["f"] is not None:
                    yield from fin_state["f"]
                    fin_state["f"] = None

            ag = attn_gen()
            mgs = [seq_gen(s_) for s_ in range(NSEQ)]
            n_attn = NSEQ * 4 * sum(2 * (4 * jq_ + 4) for jq_ in range(S // 512))
            n_ml = NSEQ * (1 + (S // 128) * 17)
            ratio = max(1, n_attn // n_ml)
            a_done, mi_ = False, 0
            while not a_done or mgs:
                if not a_done:
                    for _ in range(ratio):
                        try:
                            next(ag)
                        except StopIteration:
                            a_done = True
                            break
                if mgs:
                    g_ = mgs[mi_ % len(mgs)]
                    mi_ += 1
                    try:
                        next(g_)
                    except StopIteration:
                        mgs.remove(g_)
            precast_to(len(chunks))
            precast_to(len(chunks))
            Sx.barrier()
        if stop_after == 3:
            Sx.finish()
            return nc

        with ExitStack() as ph:
            sb, ps = mk_alloc(ph, "p4")
            wo = sb("wo", [128, 8, D], BF16)
            for k in range(8):
                Sx.dma("pool", wo[:, k, :], w_out_d[:, k, :], writes=[wo.sub(k)])
            wo_all = [wo.sub(k) for k in range(8)]
            wr = sb("wr", [128, 8, NE], F32)
            Sx.dma("sp", wr[:], w_rt_d[:, :, :], writes=[wr])
            brt = sb("brt", [128, NE], F32)
            Sx.dma("sp", brt[:], b_rt_d[0, :].partition_broadcast(128), writes=[brt])
            g1bc = sb("g1bc", [128, NSEQ, D], F32)
            A2bc = sb("A2bc", [128, NSEQ, D], F32)
            s2bc = sb("s2bc", [128, NSEQ, D], F32)
            g2n = sb("g2n", [128, D], F32)
            Sx.dma("sp", g2n[:], n2g_d[0, :].partition_broadcast(128), writes=[g2n])
            for s in range(NSEQ):
                Sx.dma("sp", g1bc[:, s, :], mod_d[s, 2 * D:3 * D].partition_broadcast(128), reads=[R_mod], writes=[g1bc])
                Sx.dma("sp", s2bc[:, s, :], mod_d[s, 3 * D:4 * D].partition_broadcast(128), reads=[R_mod], writes=[s2bc])
                Sx.dma("sp", A2bc[:, s, :], mod_d[s, 4 * D:5 * D].partition_broadcast(128), reads=[R_mod], writes=[A2bc])
                op("dve", lambda e: e.scalar_tensor_tensor(out=A2bc[:, s, :], in0=A2bc[:, s, :], scalar=1.0, in1=g2n[:], op0=ALU.add, op1=ALU.mult),
                   reads=[A2bc, g2n], writes=[A2bc])
            NW = 3
            catb = Ring([sb("catb%d" % i, [128, D], BF16) for i in range(NW)])
            xt = Ring([sb("xt%d" % i, [128, D], F32) for i in range(NW)])
            catT = Ring([sb("catT%d" % i, [128, 8, 128], BF16) for i in range(NW)])
            x1 = Ring([sb("x1_%d" % i, [128, D], F32) for i in range(NW)])
            sq4 = sb("sq4", [128, D], BF16)
            st4 = Ring([sb("st4_%d" % i, [128, 4], F32) for i in range(NW)])
            h2 = Ring([sb("h2_%d" % i, [128, D], F32) for i in range(NW)])
            h2b = Ring([sb("h2b%d" % i, [128, D], BF16) for i in range(NW)])
            h2T = Ring([sb("h2T%d" % i, [128, 8, 128], F32) for i in range(NW)])
            e4 = Ring([sb("e4_%d" % i, [128, 4], F32) for i in range(NW)])
            pT4 = Ring([ps("pT4_%d" % i, [128, 8, 128], BF16) for i in range(2)])
            pMx = Ring([ps("pMx%d" % i, [128, 512], F32) for i in range(2)])
            pR = Ring([ps("pR%d" % i, [128, 4, 128], F32) for i in range(2)])
            pL = Ring([ps("pL%d" % i, [128, 512], F32) for i in range(2)])

            def tile4(ti):
                s = (ti * 128) // S
                c_, x_ = catb.next(), xt.next()
                Sx.dma("sp", c_[:], cat_d[ti * 128:(ti + 1) * 128, :], reads=[R_cat], writes=[c_])
                Sx.dma("sp", x_[:], x_d[ti * 128:(ti + 1) * 128, :], writes=[x_])
                yield
                pt = pT4.next()
                for k in range(8):
                    op("pe", lambda e: e.transpose(out=pt[:, k, :], in_=c_[:, k * 128:(k + 1) * 128], identity=identb[:]),
                       reads=[c_, identb], writes=[pt])
                cT = catT.next()
                op("act", lambda e: e.copy(out=cT[:], in_=pt[:]), reads=[pt], writes=[cT])
                yield
                x1_ = x1.next()
                for hf in range(2):
                    pm = pMx.next()
                    for k in range(8):
                        op("pe", lambda e: e.matmul(pm[:], lhsT=cT[:, k, :], rhs=wo[:, k, hf * 512:(hf + 1) * 512], start=(k == 0),
                                                    stop=(k == 7)), reads=[cT] + wo_all, writes=[pm])
                    hs = slice(hf * 512, (hf + 1) * 512)
                    op("dve", lambda e: e.tensor_tensor(out=x1_[:, hs], in0=pm[:], in1=g1bc[:, s, hs], op=ALU.mult),
                       reads=[pm, g1bc], writes=[x1_.sub(hf)])
                    op("pool", lambda e: e.tensor_tensor(out=x1_[:, hs], in0=x1_[:, hs], in1=x_[:, hs], op=ALU.add),
                       reads=[x1_.sub(hf), x_], writes=[x1_.sub(hf)])
                    yield
                x1a = [x1_.sub(0), x1_.sub(1)]
                Sx.dma("sp", x1_d[ti * 128:(ti + 1) * 128, :], x1_[:], reads=x1a, writes=[R_x1])
                st_ = st4.next()
                op("act", lambda e: e.activation(out=sq4[:], in_=x1_[:], func=AF.Square, accum_out=st_[:, 0:1]), reads=x1a, writes=[sq4, st_])
                yield
                op("dve", lambda e: e.tensor_scalar(out=st_[:, 0:1], in0=st_[:, 0:1], scalar1=1.0 / D, scalar2=EPS, op0=ALU.mult, op1=ALU.add),
                   reads=[st_], writes=[st_])
                op("pool", lambda e: e.tensor_tensor(out=st_[:, 1:2], in0=st_[:, 0:1], in1=mhalf[:, 0:1], op=ALU.pow), reads=[st_, mhalf], writes=[st_])
                yield
                h2_ = h2.next()
                h2b_ = h2b.next()
                op("dve", lambda e: e.scalar_tensor_tensor(out=h2_[:], in0=x1_[:], scalar=st_[:, 1:2], in1=A2bc[:, s, :], op0=ALU.mult, op1=ALU.mult),
                   reads=x1a + [st_, A2bc], writes=[h2_])
                yield
                op("pool", lambda e: e.tensor_tensor(out=h2_[:], in0=h2_[:], in1=s2bc[:, s, :], op=ALU.add), reads=[h2_, s2bc], writes=[h2_])
                yield
                op("act", lambda e: e.copy(out=h2b_[:], in_=h2_[:]), reads=[h2_], writes=[h2b_])
                Sx.dma("sp", h2_d[ti * 128:(ti + 1) * 128, :], h2b_[:], reads=[h2b_], writes=[R_h2])
                hT_ = h2T.next()
                for hf in range(2):
                    pr = pR.next()
                    for k in range(4):
                        kk = hf * 4 + k
                        op("pe", lambda e: e.transpose(out=pr[:, k, :], in_=h2_[:, kk * 128:(kk + 1) * 128], identity=identf[:]),
                           reads=[h2_, identf], writes=[pr])
                    if hf == 0:
                        op("act", lambda e: e.copy(out=hT_[:, 0:4, :], in_=pr[:]), reads=[pr], writes=[hT_.sub(0)])
                    else:
                        op("dve", lambda e: e.tensor_copy(out=hT_[:, 4:8, :], in_=pr[:]), reads=[pr], writes=[hT_.sub(1)])
                    yield
                pl = pL.next()
                for k in range(8):
                    op("pe", lambda e: e.matmul(pl[:, 0:NE], lhsT=hT_[:, k, :], rhs=wr[:, k, :], start=(k == 0), stop=(k == 7)),
                       reads=[hT_.sub(0), hT_.sub(1), wr], writes=[pl])
                lgt = lg_all.sub(ti)
                op("dve", lambda e: e.tensor_tensor(out=lg_all[:, ti, :], in0=pl[:, 0:NE], in1=brt[:], op=ALU.add), reads=[pl, brt], writes=[lgt])
                t8 = top8_all.sub(ti)
                op("dve", lambda e: e.max(out=top8_all[:, ti, :], in_=lg_all[:, ti, :]), reads=[lgt], writes=[t8])
                yield
                op("dve", lambda e: e.tensor_scalar(out=st_[:, 2:3], in0=top8_all[:, ti, 0:1], scalar1=-1.0, scalar2=None, op0=ALU.mult),
                   reads=[t8], writes=[st_])
                op("dve", lambda e: e.tensor_scalar(out=M_all[:, ti, :], in0=lg_all[:, ti, :], scalar1=top8_all[:, ti, 3:4], scalar2=None,
                                                    op0=ALU.is_ge), reads=[lgt, t8], writes=[M_all.sub(ti)])
                yield
                e4_ = e4.next()
                op("act", lambda e: e.activation(out=e4_[:], in_=top8_all[:, ti, 0:4], func=AF.Exp, bias=st_[:, 2:3], accum_out=st_[:, 3:4]),
                   reads=[t8, st_], writes=[e4_, st_])
                yield
                op("dve", lambda e: e.reciprocal(out=st_[:, 3:4], in_=st_[:, 3:4]), reads=[st_], writes=[st_])
                yield
                op("dve", lambda e: e.tensor_scalar(out=G4[:, ti, :], in0=e4_[:], scalar1=st_[:, 3:4], scalar2=None, op0=ALU.mult),
                   reads=[e4_, st_], writes=[G4.sub(ti)])

            interleave((tile4(ti) for ti in range(NT)), NW)
            Sx.barrier()
        if stop_after == 4:
            Sx.finish()
            return nc

        M_res = [M_all.sub(ti) for ti in range(NT)]
        with ExitStack() as ph:
            sb, ps = mk_alloc(ph, "p5")
            pC = ps("pC", [128, 512], F32)
            pP = Ring([ps("pP%d" % i, [128, 512], F32) for i in range(4)])
            cntf = sb("cntf", [128, NE], F32)
            cnti = sb("cnti", [128, NE], I32)
            padf = sb("padf", [128, NE], F32)
            pend = sb("pend", [128, NE], F32)
            pstart = sb("pstart", [128, NE], F32)
            onesf = sb("onesf", [128, NE], F32)
            junk = sb("junk", [128, NE], F32)
            blkf = sb("blkf", [128, NBLK], F32)
            pidf = sb("pidf", [128, 1], F32)
            pidi = sb("pidi", [128, 1], I32)
            tokid = sb("tokid", [128, NT], I32)
            zer = sb("zer", [128, NROWS // 128], I32)
            for ti in range(NT):
                op("pe", lambda e: e.matmul(pC[:, 0:NE], lhsT=onesb[:], rhs=M_all[:, ti, :], start=(ti == 0), stop=(ti == NT - 1)),
                   reads=[onesb, M_res[ti]], writes=[pC])
            op("dve", lambda e: e.tensor_scalar(out=cntf[:], in0=pC[:, 0:NE], scalar1=float(BLK - 1), scalar2=None, op0=ALU.add), reads=[pC], writes=[cntf])
            op("dve", lambda e: e.tensor_copy(out=cnti[:], in_=cntf[:]), reads=[cntf], writes=[cnti])
            op("dve", lambda e: e.tensor_scalar(out=cnti[:], in0=cnti[:], scalar1=LOGB, scalar2=LOGB, op0=ALU.arith_shift_right,
                                                op1=ALU.logical_shift_left), reads=[cnti], writes=[cnti])
            op("dve", lambda e: e.tensor_copy(out=padf[:], in_=cnti[:]), reads=[cnti], writes=[padf])
            op("dve", lambda e: e.memset(onesf[:], 1.0), writes=[onesf])
            op("dve", lambda e: e.tensor_tensor_scan(out=pend[:], data0=onesf[:], data1=padf[:], initial=0.0, op0=ALU.mult, op1=ALU.add),
               reads=[onesf, padf], writes=[pend])
            op("dve", lambda e: e.tensor_tensor(out=pstart[:], in0=pend[:], in1=padf[:], op=ALU.subtract), reads=[pend, padf], writes=[pstart])
            thri = sb("thri", [128, NBLK], I32)
            thrf = sb("thrf", [128, NBLK], F32)
            cmpb = sb("cmpb", [128, NBLK, NE], F32)
            op("pool", lambda e: e.iota(thri[:], pattern=[[BLK, NBLK]], base=0, channel_multiplier=0), writes=[thri])
            op("dve", lambda e: e.tensor_copy(out=thrf[:], in_=thri[:]), reads=[thri], writes=[thrf])
            op("dve", lambda e: e.tensor_tensor(out=cmpb[:], in0=pend[:].unsqueeze(1).to_broadcast([128, NBLK, NE]),
                                                in1=thrf[:].unsqueeze(2).to_broadcast([128, NBLK, NE]), op=ALU.is_le),
               reads=[pend, thrf], writes=[cmpb])
            op("dve", lambda e: e.tensor_reduce(out=blkf[:], in_=cmpb[:], axis=AX.X, op=ALU.add), reads=[cmpb], writes=[blkf])
            op("dve", lambda e: e.tensor_scalar(out=blkf[:], in0=blkf[:], scalar1=float(NE - 1), scalar2=None, op0=ALU.min), reads=[blkf], writes=[blkf])
            op("pool", lambda e: e.iota(pidi[:], pattern=[[0, 1]], base=0, channel_multiplier=1), writes=[pidi])
            op("dve", lambda e: e.tensor_copy(out=pidf[:], in_=pidi[:]), reads=[pidi], writes=[pidf])
            wtmp = sb("wtmp", [128, NBLK], F32)
            op("dve", lambda e: e.tensor_scalar(out=wtmp[:], in0=blkf[:], scalar1=128.0, scalar2=pidf[:, 0:1], op0=ALU.mult, op1=ALU.add),
               reads=[blkf, pidf], writes=[wtmp])
            op("dve", lambda e: e.tensor_copy(out=widx[:], in_=wtmp[:]), reads=[wtmp], writes=[widx])
            op("dve", lambda e: e.tensor_copy(out=eidx[:], in_=blkf[:]), reads=[blkf], writes=[eidx])
            op("pool", lambda e: e.iota(tokid[:], pattern=[[128, NT]], base=0, channel_multiplier=1), writes=[tokid])
            op("pool", lambda e: e.memset(zer[:], 0), writes=[zer])
            Sx.dma("sp", rtok_d.rearrange("(p a) o -> p (a o)", p=128), zer[:], reads=[zer], writes=[R_rtok])
            dd = Ring([sb("dd%d" % i, [128, NE], F32) for i in range(2)])
            oh = Ring([sb("oh%d" % i, [128, 4, NE], F32) for i in range(2)])
            destf = sb("destf", [128, NT, 4], F32)
            for ti in range(NT):
                pp = pP.next()
                op("pe", lambda e: e.matmul(pp[:, 0:NE], lhsT=ustr[:], rhs=M_all[:, ti, :], start=True, stop=(ti == 0)), reads=[ustr, M_res[ti]], writes=[pp])
                for t2 in range(ti):
                    op("pe", lambda e: e.matmul(pp[:, 0:NE], lhsT=onesb[:], rhs=M_all[:, t2, :], start=False, stop=(t2 == ti - 1)),
                       reads=[onesb, M_res[t2]], writes=[pp])
                d_ = dd.next()
                op("dve", lambda e: e.tensor_tensor(out=d_[:], in0=pp[:, 0:NE], in1=pstart[:], op=ALU.add), reads=[pp, pstart], writes=[d_])
                o_ = oh.next()
                op("dve", lambda e: e.tensor_tensor(out=o_[:], in0=lg_all[:, ti, :].unsqueeze(1).to_broadcast([128, 4, NE]),
                                                     in1=top8_all[:, ti, 0:4].unsqueeze(2).to_broadcast([128, 4, NE]), op=ALU.is_equal),
                   reads=[lg_all.sub(ti), top8_all.sub(ti)], writes=[o_])
                op("dve", lambda e: e.tensor_tensor(out=o_[:], in0=o_[:], in1=d_[:].unsqueeze(1).to_broadcast([128, 4, NE]), op=ALU.mult),
                   reads=[o_, d_], writes=[o_])
                op("dve", lambda e: e.tensor_reduce(out=destf[:, ti, :], in_=o_[:], axis=AX.X, op=ALU.add), reads=[o_], writes=[destf])
            op("dve", lambda e: e.tensor_copy(out=dest_i[:], in_=destf[:]), reads=[destf], writes=[dest_i])
            for ti in range(NT):
                for k in range(4):
                    Sx.idma(rtok_d, tokid[:, ti:ti + 1], dest_i[:, ti, k:k + 1], gather=False, reads=[tokid, dest_i], writes=[R_rtok], slot="scat", shared=True)
            Sx.barrier()
        if stop_after == 5:
            Sx.finish()
            return nc

        with ExitStack() as ph:
            sb, ps = mk_alloc(ph, "p6")
            wgu = Ring([sb("wgu%d" % i, [128, 8, 2048], BF16) for i in range(2)])
            wdn = Ring([sb("wdn%d" % i, [128, 8, D], BF16) for i in range(2)])
            bgu = Ring([sb("bgu%d" % i, [128, 16], F32) for i in range(2)])
            bdn = Ring([sb("bdn%d" % i, [128, D], F32) for i in range(2)])
            bgu1 = Ring([sb("bgu1_%d" % i, [128, 8], F32) for i in range(2)])
            idx = Ring([sb("idx%d" % i, [128, JB], I32) for i in range(2)])
            xb = Ring([sb("xb%d" % i, [128, JB, D], BF16) for i in range(2)])
            xbT = Ring([sb("xbT%d" % i, [128, 8, BLK], BF16) for i in range(1)])
            actT = Ring([sb("actT%d" % i, [128, 8, BLK], BF16) for i in range(1)])
            gt_ = Ring([sb("gt%d" % i, [128, BLK], F32) for i in range(2)])
            sg_ = Ring([sb("sg%d" % i, [128, BLK], F32) for i in range(2)])
            ut_ = Ring([sb("ut%d" % i, [128, BLK], F32) for i in range(1)])
            osb = Ring([sb("osb%d" % i, [128, 512], F32) for i in range(10)])
            pX = Ring([ps("pX%d" % i, [128, 8, 128], BF16) for i in range(2)])
            pGU = Ring([ps("pGU%d" % i, [128, 512], F32) for i in range(4)])
            pO = Ring([ps("pO%d" % i, [128, 512], F32) for i in range(2)])

            def load_a(b):
                wg_, bg_, ix_, xb_ = wgu.next(), bgu.next(), idx.next(), xb.next()
                Sx.dma("sp", ix_[:], rtok_d[b * BLK:(b + 1) * BLK, :].rearrange("(p j) o -> p (j o)", p=128), reads=[R_rtok], writes=[ix_])
                for j in range(JB):
                    Sx.idma(xb_[:, j, :], h2_d, ix_[:, j:j + 1], gather=True, reads=[ix_, R_h2], writes=[xb_.sub(j)])
                Sx.idma(wg_[:].rearrange("p k n -> p (k n)"), wgub_d, widx[:, b:b + 1], gather=True, reads=[widx], writes=[wg_])
                Sx.idma(bg_[:], b_gu_d, widx[:, b:b + 1], gather=True, reads=[widx], writes=[bg_])
                return wg_, bg_, xb_

            def load_b(b):
                wd_, bd_ = wdn.next(), bdn.next()
                Sx.idma(wd_[:].rearrange("p k n -> p (k n)"), wdnb_d, widx[:, b:b + 1], gather=True, reads=[widx], writes=[wd_])
                Sx.idma(bd_[:], b_dn_d, eidx[:, b:b + 1], gather=True, reads=[eidx], writes=[bd_])
                return wd_, bd_

            def do_T(xb_):
                xT = xbT.next()
                for j in range(JB):
                    px = pX.next()
                    for k in range(8):
                        op("pe", lambda e: e.transpose(out=px[:, k, :], in_=xb_[:, j, k * 128:(k + 1) * 128], identity=identb[:]),
                           reads=[xb_.sub(j), identb], writes=[px])
                    op("act", lambda e: e.copy(out=xT[:, :, j * 128:(j + 1) * 128], in_=px[:]), reads=[px], writes=[xT.sub(j)])
                return xT

            A = {0: load_a(0)}
            Bw = {0: load_b(0)}
            if NBLK > 1:
                A[1] = load_a(1)
                Bw[1] = load_b(1)
            xT = do_T(A[0][2])
            for b in range(NBLK):
                wg_, bg_, xb_ = A.pop(b)
                wd_, bd_ = Bw.pop(b)
                wg_all = [wg_]
                wd_all = [wd_]
                xT_all = [xT.sub(j) for j in range(JB)]
                aT = actT.next()
                bg1 = bgu1.next()
                op("dve", lambda e: e.tensor_scalar(out=bg1[:], in0=bg_[:, 8:16], scalar1=1.0, scalar2=None, op0=ALU.add), reads=[bg_], writes=[bg1])
                for fc in range(8):
                    pg = pGU.next()
                    for k in range(8):
                        op("pe", lambda e: e.matmul(pg[:, 0:BLK], lhsT=wg_[:, k, fc * 128:(fc + 1) * 128], rhs=xT[:, k, :], start=(k == 0), stop=(k == 7)),
                           reads=xT_all + wg_all, writes=[pg])
                    pu = pGU.next()
                    for k in range(8):
                        op("pe", lambda e: e.matmul(pu[:, 0:BLK], lhsT=wg_[:, k, 1024 + fc * 128:1024 + (fc + 1) * 128], rhs=xT[:, k, :], start=(k == 0),
                                                    stop=(k == 7)), reads=xT_all + wg_all, writes=[pu])
                    g_, s_, u_ = gt_.next(), sg_.next(), ut_.next()
                    op("dve", lambda e: e.tensor_scalar(out=g_[:], in0=pg[:, 0:BLK], scalar1=bg_[:, fc:fc + 1], scalar2=7.0, op0=ALU.add, op1=ALU.min),
                       reads=[pg, bg_], writes=[g_])
                    op("act", lambda e: e.activation(out=s_[:], in_=g_[:], func=AF.Sigmoid, scale=1.702), reads=[g_], writes=[s_])
                    op("dve", lambda e: e.tensor_scalar(out=u_[:], in0=pu[:, 0:BLK], scalar1=bg1[:, fc:fc + 1], scalar2=8.0, op0=ALU.add, op1=ALU.min),
                       reads=[pu, bg1], writes=[u_])
                    op("pool", lambda e: e.tensor_tensor(out=g_[:], in0=g_[:], in1=s_[:], op=ALU.mult), reads=[g_, s_], writes=[g_])
                    op("dve", lambda e: e.scalar_tensor_tensor(out=aT[:, fc, :], in0=u_[:], scalar=-6.0, in1=g_[:], op0=ALU.max, op1=ALU.mult),
                       reads=[g_, u_], writes=[aT.sub(fc)])
                aT_all = [aT.sub(fc) for fc in range(8)]
                if b + 2 < NBLK:
                    A[b + 2] = load_a(b + 2)
                if b + 1 < NBLK:
                    xT = do_T(A[b + 1][2])
                for j in range(JB):
                    for hf in range(2):
                        o_ = osb.next()
                        po = pO.next()
                        for fc in range(8):
                            op("pe", lambda e: e.matmul(po[:], lhsT=aT[:, fc, j * 128:(j + 1) * 128], rhs=wd_[:, fc, hf * 512:(hf + 1) * 512],
                                                        start=(fc == 0), stop=(fc == 7)), reads=aT_all + wd_all, writes=[po])
                        op("dve", lambda e: e.tensor_tensor(out=o_[:], in0=po[:], in1=bd_[:, hf * 512:(hf + 1) * 512], op=ALU.add),
                           reads=[po, bd_], writes=[o_])
                        Sx.dma("sp", oslot_d[b * BLK:(b + 1) * BLK, :].rearrange("(p j) d -> p j d", p=128)[:, j, hf * 512:(hf + 1) * 512], o_[:],
                               reads=[o_], writes=[R_oslot])
                if b + 2 < NBLK:
                    Bw[b + 2] = load_b(b + 2)
            Sx.barrier()
        if stop_after == 6:
            Sx.finish()
            return nc

        with ExitStack() as ph:
            sb, ps = mk_alloc(ph, "p7")
            g2bc = sb("g2bc", [128, NSEQ, D], F32)
            for s in range(NSEQ):
                Sx.dma("sp", g2bc[:, s, :], mod_d[s, 5 * D:6 * D].partition_broadcast(128), reads=[R_mod], writes=[g2bc])
            NW7 = 6
            rows = Ring([sb("rows%d" % i, [128, 4, D], F32) for i in range(NW7)])
            x1t = Ring([sb("x1t%d" % i, [128, D], F32) for i in range(NW7)])
            accr = Ring([sb("accr%d" % i, [128, D], F32) for i in range(NW7)])

            def tile7(ti):
                s = (ti * 128) // S
                r_, x_ = rows.next(), x1t.next()
                for k in range(4):
                    Sx.idma(r_[:, k, :], oslot_d, dest_i[:, ti, k:k + 1], gather=True, reads=[dest_i, R_oslot], writes=[r_.sub(k)],
                            slot="L_%s_%d" % (r_.r.name, k))
                Sx.dma("sp", x_[:], x1_d[ti * 128:(ti + 1) * 128, :], reads=[R_x1], writes=[x_])
                yield
                a_ = accr.next()
                op("dve", lambda e: e.tensor_scalar(out=a_[:], in0=r_[:, 0, :], scalar1=G4[:, ti, 0:1], scalar2=None, op0=ALU.mult),
                   reads=[r_.sub(0), G4.sub(ti)], writes=[a_])
                yield
                for k in range(1, 4):
                    op("dve", lambda e: e.scalar_tensor_tensor(out=a_[:], in0=r_[:, k, :], scalar=G4[:, ti, k:k + 1], in1=a_[:], op0=ALU.mult,
                                                               op1=ALU.add), reads=[r_.sub(k), G4.sub(ti), a_], writes=[a_])
                    yield
                op("pool", lambda e: e.tensor_tensor(out=a_[:], in0=a_[:], in1=g2bc[:, s, :], op=ALU.mult), reads=[a_, g2bc], writes=[a_])
                yield
                op("dve", lambda e: e.tensor_tensor(out=a_[:], in0=a_[:], in1=x_[:], op=ALU.add), reads=[a_, x_], writes=[a_])
                Sx.dma("sp", out_d[ti * 128:(ti + 1) * 128, :], a_[:], reads=[a_], writes=[])

            interleave((tile7(ti) for ti in range(NT)), NW7)
        Sx.finish()
        build_program.stats = (Sx.ninst, Sx.nwait, dict(usage))
    return nc


def _host_layout(inputs, S):
    f = lambda a: np.ascontiguousarray(np.asarray(a, dtype=np.float32))
    kp = lambda w: np.ascontiguousarray(w.reshape(8, 128, -1).transpose(1, 0, 2))
    L = {}
    L["w_ada_l"] = kp(f(inputs["w_ada"])[0])
    L["b_ada"] = f(inputs["b_ada"])
    L["norm1_g"] = f(inputs["norm1_g"])
    L["norm2_g"] = f(inputs["norm2_g"])
    L["w_in_l"] = kp(f(inputs["w_in"])[0])
    L["conv_w"] = f(inputs["conv_w"])[0]
    L["conv_b"] = f(inputs["conv_b"])
    for n in ("b_igate", "b_fgate", "mlstm_norm_g", "q_norm_g", "k_norm_g", "lambda_q1", "lambda_k1", "lambda_q2", "lambda_k2",
              "diff_norm_g", "b_router"):
        L[n] = f(inputs[n])
    L["w_out_l"] = kp(f(inputs["w_out"])[0])
    L["w_router_l"] = kp(f(inputs["w_router"])[0])
    wgu = f(inputs["w_gu"])[0]
    wgu = wgu.reshape(NE, 8, 128, 1024, 2).transpose(0, 2, 1, 4, 3)
    L["w_gu_l"] = np.ascontiguousarray(wgu).reshape(NE * 128, 16384)
    bgu = f(inputs["b_gu"])[0].reshape(NE, 8, 128, 2).transpose(0, 2, 3, 1)
    L["b_gu_l"] = np.ascontiguousarray(bgu).reshape(NE * 128, 16)
    wdn = f(inputs["w_down"])[0].reshape(NE, 8, 128, D).transpose(0, 2, 1, 3)
    L["w_down_l"] = np.ascontiguousarray(wdn).reshape(NE * 128, 8192)
    L["b_down"] = f(inputs["b_down"])[0]
    pos = np.arange(S, dtype=np.float32)
    inv_freq = (np.float32(10000.0) ** (-np.arange(0, 64, 2, dtype=np.float32) / np.float32(64))).astype(np.float32)
    ang = (pos[:, None] * inv_freq[None, :]).astype(np.float32)
    L["cos_t"] = np.cos(ang).astype(np.float32)
    L["sin_t"] = np.sin(ang).astype(np.float32)
    return L


def run(inputs, n_cores, BLK=512, stop_after=None, debug=False):
    x = np.asarray(inputs["x"], dtype=np.float32)
    c = np.asarray(inputs["c"], dtype=np.float32)
    B, S, _ = x.shape
    NSEQ = B // n_cores
    L = _host_layout(inputs, S)
    nc = build_program(NSEQ, S, BLK, stop_after=stop_after, debug=debug)
    in_maps = []
    for i in range(n_cores):
        m = dict(L)
        m["x"] = np.ascontiguousarray(x[i * NSEQ:(i + 1) * NSEQ].reshape(NSEQ * S, D))
        m["c"] = np.ascontiguousarray(c[i * NSEQ:(i + 1) * NSEQ])
        in_maps.append(m)
    res = run_bass_kernel_spmd(nc, in_maps, core_ids=list(range(n_cores)))
    out = np.concatenate([r["out"].reshape(NSEQ, S, D) for r in res.results], axis=0)
    return out, res


def kernel(**inputs):
    out, _ = run(inputs, N_CORES, BLK=512)
    return out.astype(np.float32)
```

```python
import math
from contextlib import ExitStack

import numpy as np
import concourse.bass as bass
import concourse.mybir as mybir
from concourse.bass_utils import run_bass_kernel_spmd

F32 = mybir.dt.float32
BF16 = mybir.dt.bfloat16
I32 = mybir.dt.int32
AF = mybir.ActivationFunctionType
ALU = mybir.AluOpType
AX = mybir.AxisListType

D = 1024
MQ, MK, MV, MO, MI, MF, AQ, AK, AV = 0, 256, 512, 1024, 1536, 1540, 1544, 2056, 2568
INW = 3080
NE = 32
EPS = 1e-6
LAMBDA_INIT = 0.8 - 0.6 * math.exp(0.0)
N_CORES = 8
USE_ACT_SCALE = False


class Res:
    __slots__ = ("name", "w", "r")

    def __init__(self, name):
        self.name = name
        self.w = None
        self.r = {}


class Tile:
    def __init__(self, t, name):
        self.t = t
        self.r = Res(name)
        self.subs = {}

    def __getitem__(self, k):
        return self.t[k]

    def sub(self, k):
        if k not in self.subs:
            self.subs[k] = Res("%s.%s" % (self.r.name, k))
        return self.subs[k]

    def all(self):
        return [self.r] + list(self.subs.values())


def _res(x):
    out = []
    for a in x:
        if isinstance(a, Tile):
            out.append(a.r)
        elif isinstance(a, (list, tuple)):
            out.extend(_res(a))
        else:
            out.append(a)
    return out


class Sched:
    def __init__(self, nc, es):
        self.nc = nc
        self.es = es
        self.eng = {"pe": nc.tensor, "act": nc.scalar, "dve": nc.vector, "pool": nc.gpsimd, "sp": nc.sync}
        self.sem = {k: es.enter_context(nc.semaphore("s_" + k)) for k in self.eng}
        self.cnt = {k: 0 for k in self.eng}
        self.seen = {k: {} for k in self.eng}
        self.dsem = {}
        self.free_sems = []
        self.all_dsems = []
        self.semcnt = {}
        self.ninst = 0
        self.nwait = 0

    def _wait(self, e, ev):
        sem, val, owner = ev
        if owner == e and e == "pe":
            return
        key = id(sem)
        if self.seen[e].get(key, 0) >= val:
            return
        self.eng[e].wait_ge(sem, val)
        self.seen[e][key] = val
        self.nwait += 1

    def _deps(self, e, reads, writes):
        for r in reads:
            if r.w is not None:
                self._wait(e, r.w)
        for w in writes:
            if w.w is not None and w.w[2] != e:
                self._wait(e, w.w)
            for ev in w.r.values():
                if ev[2] != e:
                    self._wait(e, ev)

    def _commit(self, ev, reads, writes):
        for r in reads:
            r.r[ev[2]] = ev
        for w in writes:
            w.w = ev
            w.r = {}

    def op(self, e, fn, reads=(), writes=()):
        reads = _res(reads)
        writes = _res(writes)
        self._deps(e, reads, writes)
        self.cnt[e] += 1
        ev = (self.sem[e], self.cnt[e], e)
        fn(self.eng[e]).then_inc(self.sem[e], 1)
        self._commit(ev, reads, writes)
        self.ninst += 1
        return ev

    def _slot(self, slot):
        if slot not in self.dsem:
            if self.free_sems:
                sem = self.free_sems.pop()
            else:
                sem = self.es.enter_context(self.nc.semaphore("d_%d" % len(self.all_dsems)))
                self.all_dsems.append(sem)
                self.semcnt[id(sem)] = 0
            self.dsem[slot] = sem

    def _auto_slot(self, reads, writes):
        for w in writes:
            if not w.name.endswith("_d"):
                return "L_" + w.name
        return "S_" + reads[0].name

    def _dma_ev(self, slot):
        sem = self.dsem[slot]
        self.semcnt[id(sem)] += 16
        return (sem, self.semcnt[id(sem)], "dma:" + slot)

    def dma(self, q, out, in_, reads=(), writes=(), slot=None, **kw):
        reads = _res(reads)
        writes = _res(writes)
        if slot is None:
            slot = self._auto_slot(reads, writes)
        self._slot(slot)
        self._deps(q, reads, writes)
        ev = self._dma_ev(slot)
        self.eng[q].dma_start(out=out, in_=in_, **kw).then_inc(ev[0], 16)
        self._commit(ev, reads, writes)
        self.ninst += 1
        return ev

    def idma(self, out, in_, idx, gather, reads=(), writes=(), slot=None, shared=False):
        reads = _res(reads)
        writes = _res(writes)
        if slot is None:
            slot = self._auto_slot(reads, writes)
        self._slot(slot)
        if shared:
            self._deps("pool", reads, [])
            for w in writes:
                if w.w is not None and w.w[2] != "dma:" + slot:
                    self._wait("pool", w.w)
                for ev in w.r.values():
                    self._wait("pool", ev)
        else:
            self._deps("pool", reads, writes)
        ev = self._dma_ev(slot)
        off = bass.IndirectOffsetOnAxis(ap=idx, axis=0)
        if gather:
            inst = self.nc.gpsimd.indirect_dma_start(out=out, out_offset=None, in_=in_, in_offset=off)
        else:
            inst = self.nc.gpsimd.indirect_dma_start(out=out, out_offset=off, in_=in_, in_offset=None)
        inst.then_inc(ev[0], 16)
        self._commit(ev, reads, writes)
        self.ninst += 1
        return ev

    def _all_events(self):
        evs = [(self.sem[k], self.cnt[k], k) for k in self.eng if self.cnt[k] > 0]
        evs += [(sem, self.semcnt[id(sem)], "dma:*") for sem in self.all_dsems if self.semcnt[id(sem)] > 0]
        return evs

    def barrier(self):
        evs = self._all_events()
        for e in self.eng:
            for ev in evs:
                if ev[2] != e:
                    self._wait(e, ev)
        self.free_sems.extend(self.dsem.values())
        self.dsem = {}

    def finish(self):
        for ev in self._all_events():
            if ev[2] != "sp":
                self._wait("sp", ev)


def interleave(gens, width):
    active = []
    it = iter(gens)
    while True:
        while len(active) < width:
            try:
                active.append(next(it))
            except StopIteration:
                break
        if not active:
            break
        for g in list(active):
            try:
                next(g)
            except StopIteration:
                active.remove(g)


class Ring:
    def __init__(self, tiles):
        self.tiles = tiles
        self.i = 0

    def next(self):
        t = self.tiles[self.i % len(self.tiles)]
        self.i += 1
        return t


class _Stop(Exception):
    pass


def build_program(NSEQ, S, BLK, stop_after=None, debug=False):
    T = NSEQ * S
    NT = T // 128
    NT5 = T // 512
    NCHS = S // 128
    NROWS = 4 * T + NE * BLK
    NBLK = NROWS // BLK
    JB = BLK // 128
    LOGB = int(math.log2(BLK))
    assert 1 << LOGB == BLK and S % 512 == 0

    nc = bass.Bass("TRN2", target_bir_lowering=False)

    def din(name, shape, dt=F32):
        return nc.dram_tensor(name, shape, dt, kind="ExternalInput").ap()

    def dscr(name, shape, dt):
        return nc.dram_tensor(name, shape, dt, kind="ExternalOutput" if debug else "Internal").ap()

    x_d = din("x", [T, D])
    c_d = din("c", [NSEQ, D])
    w_ada_d = din("w_ada_l", [128, 8, 6 * D])
    b_ada_d = din("b_ada", [1, 6 * D])
    n1g_d = din("norm1_g", [1, D])
    n2g_d = din("norm2_g", [1, D])
    w_in_d = din("w_in_l", [128, 8, INW])
    convw_d = din("conv_w", [4, 512])
    convb_d = din("conv_b", [1, 512])
    big_d = din("b_igate", [1, 4])
    bfg_d = din("b_fgate", [1, 4])
    mng_d = din("mlstm_norm_g", [1, 512])
    qng_d = din("q_norm_g", [1, 64])
    kng_d = din("k_norm_g", [1, 64])
    lq1_d = din("lambda_q1", [1, 64])
    lk1_d = din("lambda_k1", [1, 64])
    lq2_d = din("lambda_q2", [1, 64])
    lk2_d = din("lambda_k2", [1, 64])
    dng_d = din("diff_norm_g", [1, 512])
    w_out_d = din("w_out_l", [128, 8, D])
    w_rt_d = din("w_router_l", [128, 8, NE])
    b_rt_d = din("b_router", [1, NE])
    w_gu_d = din("w_gu_l", [NE * 128, 16384])
    b_gu_d = din("b_gu_l", [NE * 128, 16])
    w_dn_d = din("w_down_l", [NE * 128, 8192])
    b_dn_d = din("b_down", [NE, D])
    cos_d = din("cos_t", [S, 32])
    sin_d = din("sin_t", [S, 32])
    out_d = nc.dram_tensor("out", [T, D], F32, kind="ExternalOutput").ap()

    mod_d = dscr("mod_s", [NSEQ, 6 * D], F32)
    mqkT_d = dscr("mqkT_s", [NSEQ, 512, S], BF16)
    mk_d = dscr("mk_s", [T, 256], BF16)
    mv_d = dscr("mv_s", [T, 512], BF16)
    mo_d = dscr("mo_s", [T, 512], BF16)
    b_d = dscr("b_s", [NSEQ, 4, S], F32)
    aqT_d = dscr("aqT_s", [NSEQ, 512, S], BF16)
    akT_d = dscr("akT_s", [NSEQ, 512, S], BF16)
    av_d = dscr("av_s", [T, 512], BF16)
    cat_d = dscr("cat_s", [T, D], BF16)
    x1_d = dscr("x1_s", [T, D], F32)
    h2_d = dscr("h2_s", [T, D], BF16)
    rtok_d = dscr("rtok_s", [NROWS, 1], I32)
    oslot_d = dscr("oslot_s", [NROWS, D], F32)
    wgub_d = nc.dram_tensor("wgub_s", [NE * 128, 16384], BF16, kind="Internal").ap()
    wdnb_d = nc.dram_tensor("wdnb_s", [NE * 128, 8192], BF16, kind="Internal").ap()
    R_wgub, R_wdnb = Res("wgub_d"), Res("wdnb_d")
    R_mod, R_mqkT, R_mk, R_mv, R_mo, R_b = (Res(n) for n in ("mod_d", "mqkT_d", "mk_d", "mv_d", "mo_d", "b_d"))
    R_aqT, R_akT, R_av, R_cat, R_x1, R_h2, R_rtok, R_oslot = (
        Res(n) for n in ("aqT_d", "akT_d", "av_d", "cat_d", "x1_d", "h2_d", "rtok_d", "oslot_d"))

    with ExitStack() as es:
        Sx = Sched(nc, es)
        op = Sx.op
        usage = {}

        def mk_alloc(stack, tag):
            usage[tag] = 0

            def sb(name, shape, dt):
                n = 1
                for v in shape[1:]:
                    n *= v
                usage[tag] += n * (2 if dt == BF16 else 4)
                return Tile(stack.enter_context(nc.sbuf_tensor(tag + "_" + name, shape, dt)), tag + "_" + name)

            def ps(name, shape, dt):
                return Tile(stack.enter_context(nc.psum_tensor(tag + "_" + name, shape, dt)), tag + "_" + name)

            return sb, ps

        gsb, gps = mk_alloc(es, "g")
        identf = gsb("identf", [128, 128], F32)
        identb = gsb("identb", [128, 128], BF16)
        onesb = gsb("onesb", [128, 128], BF16)
        ustr = gsb("ustr", [128, 128], BF16)
        tmpf = gsb("tmpf", [128, 128], F32)
        op("pool", lambda e: e.memset(identf[:], 1.0), writes=[identf])
        op("pool", lambda e: e.affine_select(out=identf[:], in_=identf[:], pattern=[[-1, 128]], compare_op=ALU.is_equal,
                                             fill=0.0, base=0, channel_multiplier=1), reads=[identf], writes=[identf])
        op("dve", lambda e: e.tensor_copy(out=identb[:], in_=identf[:]), reads=[identf], writes=[identb])
        op("dve", lambda e: e.memset(onesb[:], 1.0), writes=[onesb])
        op("pool", lambda e: e.memset(tmpf[:], 1.0), writes=[tmpf])
        op("pool", lambda e: e.affine_select(out=tmpf[:], in_=tmpf[:], pattern=[[1, 128]], compare_op=ALU.is_ge,
                                             fill=0.0, base=-1, channel_multiplier=-1), reads=[tmpf], writes=[tmpf])
        op("dve", lambda e: e.tensor_copy(out=ustr[:], in_=tmpf[:]), reads=[tmpf], writes=[ustr])
        mhalf = gsb("mhalf", [128, 16], F32)
        op("pool", lambda e: e.memset(mhalf[:], -0.5), writes=[mhalf])
        A1T = gsb("A1T", [128, NSEQ, 8], F32)
        B1T = gsb("B1T", [128, NSEQ, 8], F32)
        colA = gsb("colA", [128, NT, 4], F32)
        lg_all = gsb("lg_all", [128, NT, NE], F32)
        top8_all = gsb("top8_all", [128, NT, 8], F32)
        G4 = gsb("G4", [128, NT, 4], F32)
        M_all = gsb("M_all", [128, NT, NE], BF16)
        dest_i = gsb("dest_i", [128, NT, 4], I32)
        widx = gsb("widx", [128, NBLK], I32)
        eidx = gsb("eidx", [128, NBLK], I32)

        with ExitStack() as ph:
            sb, ps = mk_alloc(ph, "p0")
            ct = sb("ct", [NSEQ, D], F32)
            sg = sb("sg", [NSEQ, D], F32)
            condT = sb("condT", [128, 8, NSEQ], F32)
            bada = sb("bada", [NSEQ, 6 * D], F32)
            modsb = sb("modsb", [NSEQ, 6 * D], F32)
            wa = Ring([sb("wa%d" % i, [128, 8, 512], F32) for i in range(2)])
            pT0 = ps("pT0", [128, 8, NSEQ], F32)
            pM = Ring([ps("pM%d" % i, [NSEQ, 512], F32) for i in range(2)])
            Sx.dma("sp", ct[:], c_d[:, :], writes=[ct])
            Sx.dma("sp", bada[:], b_ada_d[0, :].partition_broadcast(NSEQ), writes=[bada])
            op("act", lambda e: e.activation(out=sg[:], in_=ct[:], func=AF.Sigmoid), reads=[ct], writes=[sg])
            op("dve", lambda e: e.tensor_tensor(out=sg[:], in0=sg[:], in1=ct[:], op=ALU.mult), reads=[sg, ct], writes=[sg])
            for k in range(8):
                op("pe", lambda e: e.transpose(out=pT0[:, k, :], in_=sg[0:NSEQ, k * 128:(k + 1) * 128],
                                               identity=identf[0:NSEQ, 0:NSEQ]), reads=[sg, identf], writes=[pT0])
            op("dve", lambda e: e.tensor_copy(out=condT[:], in_=pT0[:]), reads=[pT0], writes=[condT])
            for cg in range(12):
                w = wa.next()
                Sx.dma("sp", w[:], w_ada_d[:, :, cg * 512:(cg + 1) * 512], writes=[w])
                pm = pM.next()
                for k in range(8):
                    op("pe", lambda e: e.matmul(pm[:], lhsT=condT[:, k, :], rhs=w[:, k, :], start=(k == 0), stop=(k == 7)),
                       reads=[condT, w], writes=[pm])
                op("dve", lambda e: e.tensor_tensor(out=modsb[:, cg * 512:(cg + 1) * 512], in0=pm[:],
                                                    in1=bada[:, cg * 512:(cg + 1) * 512], op=ALU.add),
                   reads=[pm, bada], writes=[modsb])
            Sx.dma("sp", mod_d[:, :], modsb[:], reads=[modsb], writes=[R_mod])
            sc1 = sb("sc1", [128, NSEQ, 8], F32)
            g1T = sb("g1T", [128, 8], F32)
            for s in range(NSEQ):
                Sx.dma("sp", sc1[:, s, :], mod_d[s, D:2 * D].rearrange("(k p) -> p k", p=128), reads=[R_mod], writes=[sc1], allow_slow_non_contiguous=True)
                Sx.dma("sp", B1T[:, s, :], mod_d[s, 0:D].rearrange("(k p) -> p k", p=128), reads=[R_mod], writes=[B1T], allow_slow_non_contiguous=True)
            Sx.dma("sp", g1T[:], n1g_d[0, :].rearrange("(k p) -> p k", p=128), writes=[g1T],
                   allow_slow_non_contiguous=True)
            op("dve", lambda e: e.scalar_tensor_tensor(out=A1T[:], in0=sc1[:], scalar=1.0,
                                                       in1=g1T[:].unsqueeze(1).to_broadcast([128, NSEQ, 8]),
                                                       op0=ALU.add, op1=ALU.mult), reads=[sc1, g1T], writes=[A1T])
            Sx.barrier()
        if stop_after == 0:
            Sx.finish()
            return nc

        with ExitStack() as ph:
            sb, ps = mk_alloc(ph, "p1")
            w_sb = sb("w_in", [128, 8, INW], BF16)
            for k in range(8):
                Sx.dma("pool", w_sb[:, k, :], w_in_d[:, k, :], writes=[w_sb.sub(k)])
            cw = sb("cw", [128, 4, 4], F32)
            cb = sb("cb", [128, 4], F32)
            for cch in range(4):
                Sx.dma("sp", cw[:, cch, :], convw_d[:, cch * 128:(cch + 1) * 128].rearrange("j p -> p j"), writes=[cw], allow_slow_non_contiguous=True)
            Sx.dma("sp", cb[:], convb_d[0, :].rearrange("(c p) -> p c", p=128), writes=[cb],
                   allow_slow_non_contiguous=True)
            big = sb("big", [4, 1], F32)
            bfg = sb("bfg", [4, 1], F32)
            Sx.dma("sp", big[:], big_d.rearrange("o h -> h o"), writes=[big], allow_slow_non_contiguous=True)
            Sx.dma("sp", bfg[:], bfg_d.rearrange("o h -> h o"), writes=[bfg], allow_slow_non_contiguous=True)
            gq = sb("gq", [128, 64], F32)
            gk = sb("gk", [128, 64], F32)
            Sx.dma("sp", gq[:], qng_d[0, :].partition_broadcast(128), writes=[gq])
            Sx.dma("sp", gk[:], kng_d[0, :].partition_broadcast(128), writes=[gk])
            op("dve", lambda e: e.tensor_scalar(out=gq[:], in0=gq[:], scalar1=0.125, scalar2=None, op0=ALU.mult),
               reads=[gq], writes=[gq])
            rmask = sb("rmask", [4, 512], F32)
            op("pool", lambda e: e.memset(rmask[:], 1.0), writes=[rmask])
            for j in range(4):
                op("pool", lambda e: e.memset(rmask[:, j * 128:j * 128 + 1], 0.0), reads=[rmask], writes=[rmask])

            xt = Ring([sb("xt%d" % i, [128, D], F32) for i in range(4)])
            sqj = sb("sqj", [128, D], BF16)
            ss = Ring([sb("ss%d" % i, [128, 4], F32) for i in range(2)])
            hb = Ring([sb("hb%d" % i, [128, 4, D], BF16) for i in range(1)])
            hT = Ring([sb("hT%d" % i, [128, 8, 512], BF16) for i in range(2)])
            pre = sb("pre", [128, 4, 515], F32)
            acc = Ring([sb("acc%d" % i, [128, 512], F32) for i in range(2)])
            sig = Ring([sb("sig%d" % i, [128, 512], F32) for i in range(2)])
            qkT = Ring([sb("qkT%d" % i, [128, 4, 512], BF16) for i in range(2)])
            ktok = Ring([sb("ktok%d" % i, [128, 4, 256], BF16) for i in range(2)])
            gt = {n: sb("g_" + n, [4, 512], F32) for n in ("ip", "z", "t", "b")}
            stg = {n: Ring([sb("stg_%s%d" % (n, i), [128, 512], BF16) for i in range(2)]) for n in ("mv", "mo", "av")}
            qkx = Ring([sb("qkx%d" % i, [128, 2, 512], F32) for i in range(2)])
            qkw = {n: sb("qkw_" + n, [128, 2, 512], F32) for n in ("xn", "t1", "t2")}
            qst16 = sb("qst16", [128, 16], F32)
            gqk = sb("gqk", [128, 2, 64], F32)
            op("dve", lambda e: e.tensor_copy(out=gqk[:, 0, :], in_=gq[:]), reads=[gq], writes=[gqk])
            op("dve", lambda e: e.tensor_copy(out=gqk[:, 1, :], in_=gk[:]), reads=[gk], writes=[gqk])
            aqkb = sb("aqkb", [128, 4, 2, 512], BF16)
            aqT = {n: Ring([sb("aqT_%s%d" % (n, i), [128, 4, 512], BF16) for i in range(1)]) for n in ("q", "k")}
            cst = Ring([sb("cst%d" % i, [128, 2, 32], F32) for i in range(3)])
            pTr = Ring([ps("pTr%d" % i, [128, 2, 512], BF16) for i in range(2)])
            pMM = Ring([ps("pMM%d" % i, [128, 512], F32) for i in range(4)])
            pMi = Ring([ps("pMi%d" % i, [128, 1024], BF16) for i in range(2)])
            op("pool", lambda e: e.memset(pre[:], 0.0), writes=[pre] + [pre.sub(c_) for c_ in range(4)])

            for i in range(NT5 if stop_after != 0.05 else 0):
                s = (i * 512) // S
                tpos = (i * 512) % S
                ssi = ss.next()
                hbi = hb.next()
                hTi = hT.next()
                xts = []
                for j in range(4):
                    xj = xt.next()
                    tb = i * 512 + j * 128
                    Sx.dma("sp", xj[:], x_d[tb:tb + 128, :], writes=[xj])
                    op("act", lambda e: e.activation(out=sqj[:], in_=xj[:], func=AF.Square, accum_out=ssi[:, j:j + 1]),
                       reads=[xj], writes=[sqj, ssi])
                    xts.append(xj)
                    if j == 3:
                        op("dve", lambda e: e.tensor_scalar(out=ssi[:], in0=ssi[:], scalar1=1.0 / D, scalar2=EPS, op0=ALU.mult,
                                                            op1=ALU.add), reads=[ssi], writes=[ssi])
                        op("pool", lambda e: e.tensor_tensor(out=ssi[:], in0=ssi[:], in1=mhalf[:, 0:4], op=ALU.pow), reads=[ssi, mhalf], writes=[ssi])
                        for jj in range(4):
                            op("dve", lambda e: e.tensor_scalar(out=hbi[:, jj, :], in0=xts[jj][:], scalar1=ssi[:, jj:jj + 1],
                                                                scalar2=None, op0=ALU.mult), reads=[xts[jj], ssi],
                               writes=[hbi.sub(jj)])
                if stop_after == 0.07:
                    continue
                for kp in range(4):
                    pt = pTr.next()
                    for kk in range(2):
                        k = kp * 2 + kk
                        for j in range(4):
                            op("pe", lambda e: e.transpose(out=pt[:, kk, j * 128:(j + 1) * 128], in_=hbi[:, j, k * 128:(k + 1) * 128],
                                                           identity=identb[:]), reads=[hbi.sub(j), identb], writes=[pt])
                    for kk in range(2):
                        k = kp * 2 + kk
                        if kk == 0 and USE_ACT_SCALE:
                            op("act", lambda e: e.activation(out=hTi[:, k, :], in_=pt[:, kk, :], func=AF.Identity,
                                                             scale=A1T[:, s, k:k + 1], bias=B1T[:, s, k:k + 1]),
                               reads=[pt, A1T, B1T], writes=[hTi.sub(k)])
                        else:
                            op("dve", lambda e: e.tensor_scalar(out=hTi[:, k, :], in0=pt[:, kk, :], scalar1=A1T[:, s, k:k + 1],
                                                                scalar2=B1T[:, s, k:k + 1], op0=ALU.mult, op1=ALU.add),
                               reads=[pt, A1T, B1T], writes=[hTi.sub(k)])
                hT_all = [hTi.sub(k) for k in range(8)]
                if stop_after == 0.1:
                    continue
                w_all = [w_sb.sub(k) for k in range(8)]
                qki = qkT.next()
                for cch in range(4):
                    pm = pMM.next()
                    for k in range(8):
                        op("pe", lambda e: e.matmul(pm[:], lhsT=w_sb[:, k, cch * 128:(cch + 1) * 128], rhs=hTi[:, k, :],
                                                    start=(k == 0), stop=(k == 7)), reads=hT_all + w_all, writes=[pm])
                    prc = pre.sub(cch)
                    if tpos == 0:
                        op("pool", lambda e: e.memset(pre[:, cch, 0:3], 0.0), writes=[prc])
                    else:
                        op("pool", lambda e: e.tensor_copy(out=pre[:, cch, 0:3], in_=pre[:, cch, 512:515]), reads=[prc], writes=[prc])
                    op("act", lambda e: e.copy(out=pre[:, cch, 3:515], in_=pm[:]), reads=[pm], writes=[prc])
                    ac = acc.next()
                    sgm = sig.next()
                    op("dve", lambda e: e.tensor_scalar(out=ac[:], in0=pre[:, cch, 3:515], scalar1=cw[:, cch, 3:4],
                                                        scalar2=cb[:, cch:cch + 1], op0=ALU.mult, op1=ALU.add),
                       reads=[prc, cw, cb], writes=[ac])
                    for tap in (2, 1, 0):
                        op("dve", lambda e: e.scalar_tensor_tensor(out=ac[:], in0=pre[:, cch, tap:tap + 512],
                                                                   scalar=cw[:, cch, tap:tap + 1], in1=ac[:],
                                                                   op0=ALU.mult, op1=ALU.add), reads=[prc, cw, ac], writes=[ac])
                    op("act", lambda e: e.activation(out=sgm[:], in_=ac[:], func=AF.Sigmoid), reads=[ac], writes=[sgm])
                    if cch < 2:
                        op("dve", lambda e: e.scalar_tensor_tensor(out=qki[:, cch, :], in0=ac[:], scalar=0.125, in1=sgm[:], op0=ALU.mult,
                                                                   op1=ALU.mult), reads=[ac, sgm], writes=[qki.sub(cch)])
                    else:
                        op("pool", lambda e: e.tensor_tensor(out=qki[:, cch, :], in0=ac[:], in1=sgm[:], op=ALU.mult),
                           reads=[ac, sgm], writes=[qki.sub(cch)])
                Sx.dma("sp", mqkT_d[s, :, tpos:tpos + 512].rearrange("(c p) t -> p c t", p=128), qki[:],
                       reads=[qki.sub(c_) for c_ in range(4)], writes=[R_mqkT])
                def k_transposes():
                    kti = ktok.next()
                    pk = pMi.next()
                    for cch in (2, 3):
                        for j in range(4):
                            op("pe", lambda e: e.transpose(out=pk[:, j * 256 + (cch - 2) * 128:j * 256 + (cch - 1) * 128],
                                                           in_=qki[:, cch, j * 128:(j + 1) * 128], identity=identb[:]),
                               reads=[qki.sub(cch), identb], writes=[pk])
                    op("act", lambda e: e.copy(out=kti[:].rearrange("p j f -> p (j f)"), in_=pk[:]), reads=[pk], writes=[kti])
                    Sx.dma("sp", mk_d[i * 512:(i + 1) * 512, :].rearrange("(j p) f -> p j f", p=128), kti[:], reads=[kti],
                           writes=[R_mk])
                if stop_after == 0.2:
                    continue
                pI = pMM.next()
                for k in range(8):
                    op("pe", lambda e: e.matmul(pI[0:4, :], lhsT=w_sb[:, k, MI:MI + 4], rhs=hTi[:, k, :], start=(k == 0), stop=(k == 7)),
                       reads=hT_all + w_all, writes=[pI])
                pF = pMM.next()
                for k in range(8):
                    op("pe", lambda e: e.matmul(pF[0:4, :], lhsT=w_sb[:, k, MF:MF + 4], rhs=hTi[:, k, :], start=(k == 0), stop=(k == 7)),
                       reads=hT_all + w_all, writes=[pF])
                g = gt
                op("dve", lambda e: e.tensor_scalar(out=g["ip"][:], in0=pI[0:4, :], scalar1=big[:, 0:1], scalar2=None, op0=ALU.add),
                   reads=[pI, big], writes=[g["ip"]])
                op("dve", lambda e: e.tensor_scalar(out=g["z"][:], in0=pF[0:4, :], scalar1=bfg[:, 0:1], scalar2=None, op0=ALU.add),
                   reads=[pF, bfg], writes=[g["z"]])
                op("dve", lambda e: e.scalar_tensor_tensor(out=g["t"][:], in0=g["z"][:], scalar=-1.0, in1=g["z"][:], op0=ALU.mult, op1=ALU.max),
                   reads=[g["z"]], writes=[g["t"]])
                op("act", lambda e: e.activation(out=g["t"][:], in_=g["t"][:], func=AF.Exp, scale=-1.0), reads=[g["t"]], writes=[g["t"]])
                op("act", lambda e: e.activation(out=g["t"][:], in_=g["t"][:], func=AF.Ln, bias=1.0), reads=[g["t"]], writes=[g["t"]])
                op("dve", lambda e: e.scalar_tensor_tensor(out=g["z"][:], in0=g["z"][:], scalar=0.0, in1=g["t"][:], op0=ALU.min,
                                                           op1=ALU.subtract), reads=[g["z"], g["t"]], writes=[g["z"]])
                op("dve", lambda e: e.tensor_tensor_scan(out=g["b"][:], data0=rmask[:], data1=g["z"][:], initial=0.0, op0=ALU.mult,
                                                         op1=ALU.add), reads=[rmask, g["z"]], writes=[g["b"]])
                Sx.dma("sp", b_d[s, :, tpos:tpos + 512], g["b"][:], reads=[g["b"]], writes=[R_b])
                op("dve", lambda e: e.tensor_tensor(out=g["ip"][:], in0=g["ip"][:], in1=g["b"][:], op=ALU.subtract),
                   reads=[g["ip"], g["b"]], writes=[g["ip"]])
                def gate_transposes():
                    pG = pMM.next()
                    for j in range(4):
                        op("pe", lambda e: e.transpose(out=pG[:, j * 4:(j + 1) * 4], in_=g["ip"][0:4, j * 128:(j + 1) * 128],
                                                       identity=identf[0:4, 0:4]), reads=[g["ip"], identf], writes=[pG])
                    op("dve", lambda e: e.tensor_copy(out=colA[:, i * 4:(i + 1) * 4, :].rearrange("p j h -> p (j h)"), in_=pG[:, 0:16]),
                       reads=[pG], writes=[colA])
                if stop_after == 0.3:
                    continue
                for j in range(4):
                    tb = i * 512 + j * 128
                    cs_ = cst.next()
                    Sx.dma("sp", cs_[:, 0, :], cos_d[tpos + j * 128:tpos + (j + 1) * 128, :], writes=[cs_])
                    Sx.dma("sp", cs_[:, 1, :], sin_d[tpos + j * 128:tpos + (j + 1) * 128, :], writes=[cs_])
                    xq = qkx.next()
                    for name, off in (("mv", MV), ("mo", MO), ("av", AV), ("q", AQ), ("k", AK)):
                        pm = pMM.next()
                        for k in range(8):
                            op("pe", lambda e: e.matmul(pm[:], lhsT=hTi[:, k, j * 128:(j + 1) * 128], rhs=w_sb[:, k, off:off + 512],
                                                        start=(k == 0), stop=(k == 7)), reads=hT_all + w_all, writes=[pm])
                        if name in ("mv", "mo", "av"):
                            st_ = stg[name].next()
                            if name == "mo":
                                op("act", lambda e: e.activation(out=st_[:], in_=pm[:], func=AF.Sigmoid), reads=[pm], writes=[st_])
                            elif name == "mv":
                                op("act", lambda e: e.copy(out=st_[:], in_=pm[:]), reads=[pm], writes=[st_])
                            else:
                                op("dve", lambda e: e.tensor_copy(out=st_[:], in_=pm[:]), reads=[pm], writes=[st_])
                            dd, rr = {"mv": (mv_d, R_mv), "mo": (mo_d, R_mo), "av": (av_d, R_av)}[name]
                            Sx.dma("sp", dd[tb:tb + 128, :], st_[:], reads=[st_], writes=[rr])
                        else:
                            n_ = 0 if name == "q" else 1
                            op("act", lambda e: e.copy(out=xq[:, n_, :], in_=pm[:]), reads=[pm], writes=[xq.sub(n_)])
                    W = qkw
                    xall = [xq.sub(0), xq.sub(1)]
                    f2 = lambda t_: t_[:].rearrange("p n f -> p (n f)")
                    v3 = lambda t_: t_[:].rearrange("p n (a d) -> p (n a) d", d=64)
                    v4 = lambda t_: t_[:].rearrange("p n (a two d) -> p (n a) two d", two=2, d=32)
                    op("pool", lambda e: e.tensor_tensor(out=f2(W["t1"]), in0=f2(xq), in1=f2(xq), op=ALU.mult), reads=xall, writes=[W["t1"]])
                    op("dve", lambda e: e.tensor_reduce(out=qst16[:], in_=v3(W["t1"]), axis=AX.X, op=ALU.add), reads=[W["t1"]], writes=[qst16])
                    op("dve", lambda e: e.tensor_scalar(out=qst16[:], in0=qst16[:], scalar1=1.0 / 64, scalar2=EPS, op0=ALU.mult, op1=ALU.add),
                       reads=[qst16], writes=[qst16])
                    op("pool", lambda e: e.tensor_tensor(out=qst16[:], in0=qst16[:], in1=mhalf[:, 0:16], op=ALU.pow), reads=[qst16, mhalf], writes=[qst16])
                    op("dve", lambda e: e.tensor_tensor(out=v3(W["xn"]), in0=v3(xq), in1=qst16[:].unsqueeze(2).to_broadcast([128, 16, 64]), op=ALU.mult),
                       reads=xall + [qst16], writes=[W["xn"]])
                    g4 = lambda t_: t_[:].rearrange("p n (a d) -> p n a d", d=64)
                    op("pool", lambda e: e.tensor_tensor(out=g4(W["xn"]), in0=g4(W["xn"]), in1=gqk[:].unsqueeze(2).to_broadcast([128, 2, 8, 64]), op=ALU.mult),
                       reads=[W["xn"], gqk], writes=[W["xn"]])
                    cosb = cs_[:, 0, :].unsqueeze(1).to_broadcast([128, 16, 32])
                    sinb = cs_[:, 1, :].unsqueeze(1).to_broadcast([128, 16, 32])
                    xn4, t14, t24 = v4(W["xn"]), v4(W["t1"]), v4(W["t2"])
                    o4 = aqkb[:, j, :, :].rearrange("p n (a two d) -> p (n a) two d", two=2, d=32)
                    for two in range(2):
                        op("dve", lambda e: e.tensor_tensor(out=t14[:, :, two, :], in0=xn4[:, :, two, :], in1=cosb, op=ALU.mult),
                           reads=[W["xn"], cs_], writes=[W["t1"]])
                        op("pool", lambda e: e.tensor_tensor(out=t24[:, :, two, :], in0=xn4[:, :, 1 - two, :], in1=sinb, op=ALU.mult),
                           reads=[W["xn"], cs_], writes=[W["t2"]])
                    op("dve", lambda e: e.tensor_tensor(out=o4[:, :, 0, :], in0=t14[:, :, 0, :], in1=t24[:, :, 0, :], op=ALU.subtract),
                       reads=[W["t1"], W["t2"]], writes=[aqkb.sub(j)])
                    op("pool", lambda e: e.tensor_tensor(out=o4[:, :, 1, :], in0=t14[:, :, 1, :], in1=t24[:, :, 1, :], op=ALU.add),
                       reads=[W["t1"], W["t2"]], writes=[aqkb.sub(j)])
                k_transposes()
                gate_transposes()
                for name in ("q", "k"):
                    aT = aqT[name].next()
                    n_ = 0 if name == "q" else 1
                    for fc in range(4):
                        pa = pMi.next()
                        for j in range(4):
                            op("pe", lambda e: e.transpose(out=pa[:, j * 128:(j + 1) * 128], in_=aqkb[:, j, n_, fc * 128:(fc + 1) * 128],
                                                           identity=identb[:]), reads=[aqkb.sub(j), identb], writes=[pa])
                        if fc % 2 == 0:
                            op("act", lambda e: e.copy(out=aT[:, fc, :], in_=pa[:, 0:512]), reads=[pa], writes=[aT.sub(fc)])
                        else:
                            op("dve", lambda e: e.tensor_copy(out=aT[:, fc, :], in_=pa[:, 0:512]), reads=[pa], writes=[aT.sub(fc)])
                    dd, rr = (aqT_d, R_aqT) if name == "q" else (akT_d, R_akT)
                    Sx.dma("sp", dd[s, :, tpos:tpos + 512].rearrange("(c p) t -> p c t", p=128), aT[:],
                           reads=[aT.sub(c_) for c_ in range(4)], writes=[rr])
            Sx.barrier()
        if stop_after is not None and stop_after <= 1:
            Sx.finish()
            return nc

        with ExitStack() as ph:
            sb, ps = mk_alloc(ph, "p23")
            gm = sb("gm", [128, 512], F32)
            Sx.dma("sp", gm[:], mng_d[0, :].partition_broadcast(128), writes=[gm])
            Dm = Ring([sb("Dm%d" % i, [128, 128], F32) for i in range(3)])
            ATr = Ring([sb("AT%d" % i, [128, 128], BF16) for i in range(3)])
            ebr = Ring([sb("eb%d" % i, [128, 128], F32) for i in range(3)])
            qsr = Ring([sb("qs%d" % i, [128, 128], BF16) for i in range(3)])
            dnr = Ring([sb("dn%d" % i, [128, 2], F32) for i in range(4)])
            hsq = sb("hsq", [128, 4, 128], F32)
            pSCs = [ps("pSC%d" % i, [128, 512], F32) for i in range(NSEQ)]


            def seq_gen(s):
                qTr = Ring([sb("qT%d_%d" % (s, i), [128, 2, 512], BF16) for i in range(2)])
                kTr = Ring([sb("kT%d_%d" % (s, i), [128, 2, 512], BF16) for i in range(2)])
                ktr = Ring([sb("kt%d_%d" % (s, i), [128, 256], BF16) for i in range(2)])
                v1r = Ring([sb("v1%d_%d" % (s, i), [128, 4, 129], BF16) for i in range(2)])
                bbr = Ring([sb("bb%d_%d" % (s, i), [128, 4, 128], F32) for i in range(2)])
                mor = Ring([sb("mo%d_%d" % (s, i), [128, 512], BF16) for i in range(2)])
                for t_ in v1r.tiles:
                    op("pool", lambda e: e.memset(t_[:], 1.0), writes=[t_])
                Cn = sb("Cn%d" % s, [128, 2, 129], F32)
                Cnb = sb("Cnb%d" % s, [128, 2, 129], BF16)
                wcr = Ring([sb("wc%d_%d" % (s, i), [128, 2], F32) for i in range(4)])
                Vwr = Ring([sb("Vw%d_%d" % (s, i), [128, 128], BF16) for i in range(3)])
                hraw = Ring([sb("hraw%d_%d" % (s, i), [128, 4, 128], F32) for i in range(2)])
                hst = Ring([sb("hst%d_%d" % (s, i), [128, 4], F32) for i in range(2)])
                hn = Ring([sb("hn%d_%d" % (s, i), [128, 512], F32) for i in range(1)])
                hob = Ring([sb("hob%d_%d" % (s, i), [128, 512], BF16) for i in range(2)])
                def load_grp(gi):
                    tpos = gi * 512
                    q_, k_ = qTr.next(), kTr.next()
                    Sx.dma("sp", q_[:], mqkT_d[s, 0:256, tpos:tpos + 512].rearrange("(c p) t -> p c t", p=128), reads=[R_mqkT], writes=[q_])
                    Sx.dma("sp", k_[:], mqkT_d[s, 256:512, tpos:tpos + 512].rearrange("(c p) t -> p c t", p=128), reads=[R_mqkT], writes=[k_])
                    return q_, k_

                def load_chunk(c):
                    tb = s * S + c * 128
                    kt_, v_, bb_, mo_ = ktr.next(), v1r.next(), bbr.next(), mor.next()
                    Sx.dma("sp", kt_[:], mk_d[tb:tb + 128, :], reads=[R_mk], writes=[kt_])
                    Sx.dma("sp", v_[:, :, 0:128], mv_d[tb:tb + 128, :].rearrange("p (h d) -> p h d", d=128), reads=[R_mv], writes=[v_])
                    Sx.dma("sp", bb_[:], b_d[s, :, c * 128:(c + 1) * 128].partition_broadcast(128), reads=[R_b], writes=[bb_])
                    Sx.dma("sp", mo_[:], mo_d[tb:tb + 128, :], reads=[R_mo], writes=[mo_])
                    return kt_, v_, bb_, mo_

                nxt = load_grp(0)
                nxc = load_chunk(0)
                yield
                for gi in range(S // 512):
                    i = s * (S // 512) + gi
                    q_, k_ = nxt
                    if gi + 1 < S // 512:
                        nxt = load_grp(gi + 1)
                    tpos = gi * 512
                    if tpos == 0:
                        op("pool", lambda e: e.memset(Cn[:], 0.0), writes=[Cn])
                        op("pool", lambda e: e.memset(Cnb[:], 0.0), writes=[Cnb])
                    for j in range(4):
                        ch = i * 4 + j
                        tsl = slice(j * 128, (j + 1) * 128)
                        c128 = slice(0, 128)
                        kt_, v_, bb_, mo_ = nxc
                        if gi * 4 + j + 1 < S // 128:
                            nxc = load_chunk(gi * 4 + j + 1)
                        hr = hraw.next()
                        for h in range(4):
                            base, hp = (h % 2) * 64, h // 2
                            psl = slice(base, base + 64)
                            pst = pSC = pSCs[s]
                            op("pe", lambda e: e.matmul(pst[:, 0:128], lhsT=k_[psl, hp, tsl], rhs=q_[psl, hp, tsl], start=True, stop=True),
                               reads=[k_, q_], writes=[pst])
                            yield
                            dm = Dm.next()
                            op("act", lambda e: e.activation(out=dm[:], in_=bb_[:, h, c128], func=AF.Exp, bias=colA[:, ch, h:h + 1]),
                               reads=[bb_, colA], writes=[dm])
                            op("pool", lambda e: e.affine_select(out=dm[:], in_=dm[:], pattern=[[1, 128]], compare_op=ALU.is_ge, fill=0.0,
                                                                 base=0, channel_multiplier=-1), reads=[dm], writes=[dm])
                            at = ATr.next()
                            op("dve", lambda e: e.tensor_tensor(out=at[:], in0=pst[:, 0:128], in1=dm[:], op=ALU.mult), reads=[pst, dm], writes=[at])
                            eb = ebr.next()
                            op("act", lambda e: e.activation(out=eb[:], in_=bb_[:, h, c128], func=AF.Exp), reads=[bb_], writes=[eb])
                            qs = qsr.next()
                            op("dve", lambda e: e.tensor_tensor(out=qs[psl, :], in0=q_[psl, hp, tsl], in1=eb[psl, :], op=ALU.mult),
                               reads=[q_, eb], writes=[qs])
                            if h % 2 == 0:
                                wc = wcr.next()
                                kw = Vwr.next()
                                for hh in (h, h + 1):
                                    glast = bb_[:, hh, 127:128]
                                    op("act", lambda e: e.activation(out=wc[:, hh - h:hh - h + 1], in_=colA[:, ch, hh:hh + 1], func=AF.Exp, bias=glast),
                                       reads=[colA, bb_], writes=[wc])
                                op("dve", lambda e: e.tensor_tensor(out=kw[:].rearrange("p (a d) -> p a d", d=64),
                                                                    in0=kt_[:, hp * 128:(hp + 1) * 128].rearrange("p (a d) -> p a d", d=64),
                                                                    in1=wc[:].unsqueeze(2).to_broadcast([128, 2, 64]), op=ALU.mult),
                                   reads=[kt_, wc], writes=[kw])
                            yield
                            pnd = pSC
                            op("pe", lambda e: e.matmul(pSC[:, 128:257], lhsT=at[:], rhs=v_[:, h, :], start=True, stop=False),
                               reads=[at, v_], writes=[pnd])
                            op("pe", lambda e: e.matmul(pSC[:, 128:257], lhsT=qs[psl, :], rhs=Cnb[psl, hp, :], start=False, stop=True),
                               reads=[qs, Cnb.sub(h)], writes=[pnd])
                            pcl = pSC
                            op("pe", lambda e: e.matmul(pSC[:, 257:386], lhsT=kw[:], rhs=v_[:, h, :], start=True, stop=True),
                               reads=[kw, v_], writes=[pcl])
                            yield
                            op("dve", lambda e: e.scalar_tensor_tensor(out=Cn[psl, hp, :], in0=Cn[psl, hp, :], scalar=eb[psl, 127:128],
                                                                       in1=pSC[psl, 257:386], op0=ALU.mult, op1=ALU.add),
                               reads=[Cn.sub(h), eb, pcl], writes=[Cn.sub(h)])
                            op("act", lambda e: e.copy(out=Cnb[psl, hp, :], in_=Cn[psl, hp, :]), reads=[Cn.sub(h)], writes=[Cnb.sub(h)])
                            dn = dnr.next()
                            op("dve", lambda e: e.tensor_scalar(out=dn[:, 1:2], in0=pSC[:, 256:257], scalar1=-1.0, scalar2=None, op0=ALU.mult),
                               reads=[pnd], writes=[dn])
                            op("dve", lambda e: e.scalar_tensor_tensor(out=dn[:, 0:1], in0=pSC[:, 256:257], scalar=1.0, in1=dn[:, 1:2], op0=ALU.max, op1=ALU.max),
                               reads=[pnd, dn], writes=[dn])
                            op("dve", lambda e: e.reciprocal(out=dn[:, 1:2], in_=dn[:, 0:1]), reads=[dn], writes=[dn])
                            op("dve", lambda e: e.tensor_scalar(out=hr[:, h, :], in0=pSC[:, 128:256], scalar1=dn[:, 1:2], scalar2=None, op0=ALU.mult),
                               reads=[pnd, dn], writes=[hr.sub(h)])
                            yield
                        hr_all = [hr.sub(h) for h in range(4)]
                        st_ = hst.next()
                        op("pool", lambda e: e.tensor_tensor(out=hsq[:], in0=hr[:], in1=hr[:], op=ALU.mult), reads=hr_all, writes=[hsq])
                        op("dve", lambda e: e.tensor_reduce(out=st_[:], in_=hsq[:], axis=AX.X, op=ALU.add), reads=[hsq], writes=[st_])
                        op("dve", lambda e: e.tensor_scalar(out=st_[:], in0=st_[:], scalar1=1.0 / 128, scalar2=EPS, op0=ALU.mult, op1=ALU.add),
                           reads=[st_], writes=[st_])
                        op("pool", lambda e: e.tensor_tensor(out=st_[:], in0=st_[:], in1=mhalf[:, 0:4], op=ALU.pow), reads=[st_, mhalf], writes=[st_])
                        hn_ = hn.next()
                        ho_ = hob.next()
                        op("dve", lambda e: e.tensor_tensor(out=hn_[:].rearrange("p (h d) -> p h d", d=128), in0=hr[:],
                                                            in1=st_[:].unsqueeze(2).to_broadcast([128, 4, 128]), op=ALU.mult),
                           reads=hr_all + [st_], writes=[hn_])
                        op("pool", lambda e: e.tensor_tensor(out=hn_[:], in0=hn_[:], in1=gm[:], op=ALU.mult), reads=[hn_, gm], writes=[hn_])
                        op("pool", lambda e: e.tensor_tensor(out=ho_[:], in0=hn_[:], in1=mo_[:], op=ALU.mult), reads=[hn_, mo_], writes=[ho_])
                        tb = i * 512 + j * 128
                        Sx.dma("sp", cat_d[tb:tb + 128, 0:512], ho_[:], reads=[ho_], writes=[R_cat])
                        yield

            NB = S // 128
            NQT = S // 512
            lam4 = sb("lam4", [128, 4, 64], F32)
            lamj = sb("lamj", [128, 64], F32)
            lams = sb("lams", [128, 4], F32)
            for n_, d_ in enumerate((lq1_d, lk1_d, lq2_d, lk2_d)):
                Sx.dma("sp", lam4[:, n_, :], d_[0, :].partition_broadcast(128), writes=[lam4])
            for n_ in range(2):
                op("dve", lambda e: e.tensor_tensor(out=lamj[:], in0=lam4[:, 2 * n_, :], in1=lam4[:, 2 * n_ + 1, :], op=ALU.mult),
                   reads=[lam4], writes=[lamj])
                op("dve", lambda e: e.tensor_reduce(out=lams[:, n_:n_ + 1], in_=lamj[:], axis=AX.X, op=ALU.add), reads=[lamj], writes=[lams])
            op("act", lambda e: e.activation(out=lams[:, 0:2], in_=lams[:, 0:2], func=AF.Exp), reads=[lams], writes=[lams])
            op("dve", lambda e: e.tensor_tensor(out=lams[:, 2:3], in0=lams[:, 1:2], in1=lams[:, 0:1], op=ALU.subtract), reads=[lams], writes=[lams])
            op("dve", lambda e: e.tensor_scalar(out=lams[:, 2:3], in0=lams[:, 2:3], scalar1=-LAMBDA_INIT, scalar2=None, op0=ALU.add),
               reads=[lams], writes=[lams])
            dg = sb("dg", [128, 512], F32)
            Sx.dma("sp", dg[:], dng_d[0, :].partition_broadcast(128), writes=[dg])
            op("dve", lambda e: e.tensor_scalar(out=dg[:], in0=dg[:], scalar1=1.0 - LAMBDA_INIT, scalar2=None, op0=ALU.mult),
               reads=[dg], writes=[dg])
            qTa = Ring([sb("qTa%d" % i, [128, S], BF16) for i in range(2)])
            kTa = Ring([sb("kTa%d" % i, [128, S], BF16) for i in range(2)])
            v1a = Ring([sb("v1a%d" % i, [128, NB, 129], BF16) for i in range(2)])
            for t_ in v1a.tiles:
                op("pool", lambda e: e.memset(t_[:], 1.0), writes=[t_])
            Pr = Ring([sb("P%d" % i, [128, 512], BF16) for i in range(6)])
            rcr = Ring([sb("rc%d" % i, [128, 4], F32) for i in range(8)])
            t1r = Ring([sb("t1_%d" % i, [128, 128], F32) for i in range(4)])
            o_r = Ring([sb("o_%d" % i, [128, 128], F32) for i in range(8)])
            osqr = Ring([sb("osq%d" % i, [128, 128], F32) for i in range(4)])
            har = Ring([sb("ha%d" % i, [128, 4, 128], BF16) for i in range(2)])
            accS = Ring([[[sb("accS%d_%d%d" % (i, m, gp), [128, 2, 129], F32) for gp in range(2)] for m in range(2)] for i in range(2)])
            pS = Ring([ps("pS%d" % i, [128, 512], F32) for i in range(2)])
            pAcc = [[ps("pA%d%d" % (m, gp), [128, 2, 129], F32) for gp in range(2)] for m in range(2)]

            wstg = Ring([sb("wstg%d" % i, [128, 4096], BF16) for i in range(4)])
            chunks = []
            for e_ in range(NE):
                rs = slice(e_ * 128, (e_ + 1) * 128)
                for c0_ in range(0, 16384, 4096):
                    chunks.append((w_gu_d[rs, c0_:c0_ + 4096], wgub_d[rs, c0_:c0_ + 4096]))
                for c0_ in range(0, 8192, 4096):
                    chunks.append((w_dn_d[rs, c0_:c0_ + 4096], wdnb_d[rs, c0_:c0_ + 4096]))
            pc_state = {"ld": 0, "st": 0, "tiles": [], "tick": 0}
            PC_LAG = 2

            def precast_store():
                c_ = pc_state["st"]
                Sx.dma("sp", chunks[c_][1], pc_state["tiles"][c_][:], reads=[pc_state["tiles"][c_]], writes=[])
                pc_state["st"] += 1

            def precast_tick():
                if pc_state["st"] < pc_state["ld"] and pc_state["ld"] - pc_state["st"] >= PC_LAG:
                    precast_store()
                if pc_state["ld"] < len(chunks):
                    c_ = pc_state["ld"]
                    t_ = wstg.next()
                    Sx.dma("pool", t_[:], chunks[c_][0], writes=[t_])
                    pc_state["tiles"].append(t_)
                    pc_state["ld"] += 1
                elif pc_state["st"] < pc_state["ld"]:
                    precast_store()

            def precast_to(n_ld):
                while pc_state["ld"] < min(n_ld, len(chunks)) or (n_ld >= len(chunks) and pc_state["st"] < len(chunks)):
                    precast_tick()

            def load_head(sh):
                s, h = divmod(sh, 4)
                q_, k_, v_ = qTa.next(), kTa.next(), v1a.next()
                z = sh % 2
                Sx.dma("sp", q_[:], aqT_d[s, h * 128:(h + 1) * 128, :], reads=[R_aqT], writes=[q_])
                Sx.dma("sp", k_[:], akT_d[s, h * 128:(h + 1) * 128, :], reads=[R_akT], writes=[k_])
                Sx.dma("sp", v_[:, :, 0:128], av_d[s * S:(s + 1) * S, h * 128:(h + 1) * 128].rearrange("(kb p) d -> p kb d", p=128),
                       reads=[R_av], writes=[v_])
                return q_, k_, v_

            n_attn_steps = NSEQ * 4 * sum(2 * (4 * jq_ + 4) for jq_ in range(S // 512))

            def attn_gen():
                fin_state = {"f": None}
                nxt = load_head(0)
                for sh in range(NSEQ * 4):
                    s, h = divmod(sh, 4)
                    q_, k_, v_ = nxt
                    if sh + 1 < NSEQ * 4:
                        nxt = load_head(sh + 1)
                    for jq in range(NQT):
                        it_ = sh * NQT + jq
                        qend = (jq + 1) * 512
                        nkb = 4 * jq + 4
                        steps = []
                        for kb in range(nkb):
                            qlo = max(jq * 512, kb * 128)
                            for m in range(2):
                                steps.append((kb, m, qlo, qend - qlo, (qlo - jq * 512) // 128))

                        def issue_st(st):
                            kb, m, qlo, ncols, g0 = st
                            msl = slice(m * 64, m * 64 + 64)
                            pst = pS.next()
                            op("pe", lambda e: e.matmul(pst[:, 0:ncols], lhsT=k_[msl, kb * 128:(kb + 1) * 128], rhs=q_[msl, qlo:qend],
                                                        start=True, stop=True), reads=[k_, q_], writes=[pst])
                            P = Pr.next()
                            op("act", lambda e: e.activation(out=P[:, 0:ncols], in_=pst[:, 0:ncols], func=AF.Exp), reads=[pst], writes=[P])
                            if kb >= 4 * jq:
                                op("dve", lambda e: e.memset(P[64:128, 0:64], 0.0), reads=[P], writes=[P])
                            return P

                        def issue_pv(st, P):
                            kb, m, qlo, ncols, g0 = st
                            for g in range(g0, 4):
                                c0 = (g - g0) * 128
                                pa = pAcc[m][g // 2]
                                op("pe", lambda e: e.matmul(pa[:, g % 2, :], lhsT=P[:, c0:c0 + 128], rhs=v_[:, kb, :],
                                                            start=(kb == 0 and g % 2 == 0), stop=(kb == 4 * jq + g),
                                                            skip_group_check=True), reads=[P, v_], writes=[pa])

                        pend_ = []
                        for si_, st in enumerate(steps):
                            pend_.append((st, issue_st(st)))
                            if si_ == 2 and fin_state["f"] is not None:
                                yield from fin_state["f"]
                                fin_state["f"] = None
                            if len(pend_) > 2:
                                issue_pv(*pend_.pop(0))
                            pc_state["tick"] += 1
                            if pc_state["tick"] * len(chunks) // n_attn_steps > pc_state["ld"]:
                                precast_tick()
                            yield
                        if fin_state["f"] is not None:
                            yield from fin_state["f"]
                            fin_state["f"] = None
                        while pend_:
                            issue_pv(*pend_.pop(0))

                        def finalize(s=s, h=h, jq=jq):
                            ha_ = har.next()
                            acs = accS.next()
                            for m_ in range(2):
                                for gp_ in range(2):
                                    op("dve", lambda e: e.tensor_copy(out=acs[m_][gp_][:], in_=pAcc[m_][gp_][:]), reads=[pAcc[m_][gp_]], writes=[acs[m_][gp_]])
                            yield
                            rcs, os_, sqs = [], [], []
                            for g in range(4):
                                a0 = acs[0][g // 2]
                                a1 = acs[1][g // 2]
                                rc = rcr.next()
                                op("dve", lambda e: e.reciprocal(out=rc[:, 0:1], in_=a0[:, g % 2, 128:129]), reads=[a0], writes=[rc])
                                op("dve", lambda e: e.reciprocal(out=rc[:, 1:2], in_=a1[:, g % 2, 128:129]), reads=[a1], writes=[rc])
                                op("dve", lambda e: e.tensor_tensor(out=rc[:, 1:2], in0=rc[:, 1:2], in1=lams[:, 2:3], op=ALU.mult), reads=[rc, lams], writes=[rc])
                                t1 = t1r.next()
                                o_ = o_r.next()
                                op("dve", lambda e: e.tensor_scalar(out=t1[:], in0=a1[:, g % 2, 0:128], scalar1=rc[:, 1:2], scalar2=None, op0=ALU.mult),
                                   reads=[a1, rc], writes=[t1])
                                op("dve", lambda e: e.scalar_tensor_tensor(out=o_[:], in0=a0[:, g % 2, 0:128], scalar=rc[:, 0:1], in1=t1[:],
                                                                           op0=ALU.mult, op1=ALU.add), reads=[a0, rc, t1], writes=[o_])
                                rcs.append(rc)
                                os_.append(o_)
                            yield
                            for g in range(4):
                                sq_ = osqr.next()
                                op("pool", lambda e: e.tensor_tensor(out=sq_[:], in0=os_[g][:], in1=os_[g][:], op=ALU.mult), reads=[os_[g]], writes=[sq_])
                                sqs.append(sq_)
                            yield
                            for g in range(4):
                                rc = rcs[g]
                                op("dve", lambda e: e.tensor_reduce(out=rc[:, 2:3], in_=sqs[g][:], axis=AX.X, op=ALU.add), reads=[sqs[g]], writes=[rc])
                                op("dve", lambda e: e.tensor_scalar(out=rc[:, 2:3], in0=rc[:, 2:3], scalar1=1.0 / 128, scalar2=EPS, op0=ALU.mult, op1=ALU.add),
                                   reads=[rc], writes=[rc])
                            yield
                            for g in range(4):
                                rc = rcs[g]
                                op("pool", lambda e: e.tensor_tensor(out=rc[:, 3:4], in0=rc[:, 2:3], in1=mhalf[:, 0:1], op=ALU.pow), reads=[rc, mhalf], writes=[rc])
                            yield
                            for g in range(4):
                                rc = rcs[g]
                                op("dve", lambda e: e.scalar_tensor_tensor(out=ha_[:, g, :], in0=os_[g][:], scalar=rc[:, 3:4], in1=dg[:, h * 128:(h + 1) * 128],
                                                                           op0=ALU.mult, op1=ALU.mult), reads=[os_[g], rc, dg], writes=[ha_.sub(g)])
                            t0 = s * S + jq * 512
                            Sx.dma("sp", cat_d[t0:t0 + 512, 512 + h * 128:512 + (h + 1) * 128].rearrange("(g p) d -> p g d", p=128), ha_[:],
                                   reads=[ha_.sub(g) for g in range(4)], writes=[R_cat])

                        fin_state["f"] = finalize()
                if fin_state["f"] is not None:
                    yield from fin_state["f"]
                    fin_state["f"] = None

            ag = attn_gen()
            mgs = [seq_gen(s_) for s_ in range(NSEQ)]
            n_attn = NSEQ * 4 * sum(2 * (4 * jq_ + 4) for jq_ in range(S // 512))
            n_ml = NSEQ * (1 + (S // 128) * 17)
            ratio = max(1, n_attn // n_ml)
            a_done, mi_ = False, 0
            while not a_done or mgs:
                if not a_done:
                    for _ in range(ratio):
                        try:
                            next(ag)
                        except StopIteration:
                            a_done = True
                            break
                if mgs:
                    g_ = mgs[mi_ % len(mgs)]
                    mi_ += 1
                    try:
                        next(g_)
                    except StopIteration:
                        mgs.remove(g_)
            precast_to(len(chunks))
            precast_to(len(chunks))
            Sx.barrier()
        if stop_after == 3:
            Sx.finish()
            return nc

        with ExitStack() as ph:
            sb, ps = mk_alloc(ph, "p4")
            wo = sb("wo", [128, 8, D], BF16)
            for k in range(8):
                Sx.dma("pool", wo[:, k, :], w_out_d[:, k, :], writes=[wo.sub(k)])
            wo_all = [wo.sub(k) for k in range(8)]
            wr = sb("wr", [128, 8, NE], F32)
            Sx.dma("sp", wr[:], w_rt_d[:, :, :], writes=[wr])
            brt = sb("brt", [128, NE], F32)
            Sx.dma("sp", brt[:], b_rt_d[0, :].partition_broadcast(128), writes=[brt])
            g1bc = sb("g1bc", [128, NSEQ, D], F32)
            A2bc = sb("A2bc", [128, NSEQ, D], F32)
            s2bc = sb("s2bc", [128, NSEQ, D], F32)
            g2n = sb("g2n", [128, D], F32)
            Sx.dma("sp", g2n[:], n2g_d[0, :].partition_broadcast(128), writes=[g2n])
            for s in range(NSEQ):
                Sx.dma("sp", g1bc[:, s, :], mod_d[s, 2 * D:3 * D].partition_broadcast(128), reads=[R_mod], writes=[g1bc])
                Sx.dma("sp", s2bc[:, s, :], mod_d[s, 3 * D:4 * D].partition_broadcast(128), reads=[R_mod], writes=[s2bc])
                Sx.dma("sp", A2bc[:, s, :], mod_d[s, 4 * D:5 * D].partition_broadcast(128), reads=[R_mod], writes=[A2bc])
                op("dve", lambda e: e.scalar_tensor_tensor(out=A2bc[:, s, :], in0=A2bc[:, s, :], scalar=1.0, in1=g2n[:], op0=ALU.add, op1=ALU.mult),
                   reads=[A2bc, g2n], writes=[A2bc])
            NW = 3
            catb = Ring([sb("catb%d" % i, [128, D], BF16) for i in range(NW)])
            xt = Ring([sb("xt%d" % i, [128, D], F32) for i in range(NW)])
            catT = Ring([sb("catT%d" % i, [128, 8, 128], BF16) for i in range(NW)])
            x1 = Ring([sb("x1_%d" % i, [128, D], F32) for i in range(NW)])
            sq4 = sb("sq4", [128, D], BF16)
            st4 = Ring([sb("st4_%d" % i, [128, 4], F32) for i in range(NW)])
            h2 = Ring([sb("h2_%d" % i, [128, D], F32) for i in range(NW)])
            h2b = Ring([sb("h2b%d" % i, [128, D], BF16) for i in range(NW)])
            h2T = Ring([sb("h2T%d" % i, [128, 8, 128], F32) for i in range(NW)])
            e4 = Ring([sb("e4_%d" % i, [128, 4], F32) for i in range(NW)])
            pT4 = Ring([ps("pT4_%d" % i, [128, 8, 128], BF16) for i in range(2)])
            pMx = Ring([ps("pMx%d" % i, [128, 512], F32) for i in range(2)])
            pR = Ring([ps("pR%d" % i, [128, 4, 128], F32) for i in range(2)])
            pL = Ring([ps("pL%d" % i, [128, 512], F32) for i in range(2)])

            def tile4(ti):
                s = (ti * 128) // S
                c_, x_ = catb.next(), xt.next()
                Sx.dma("sp", c_[:], cat_d[ti * 128:(ti + 1) * 128, :], reads=[R_cat], writes=[c_])
                Sx.dma("sp", x_[:], x_d[ti * 128:(ti + 1) * 128, :], writes=[x_])
                yield
                pt = pT4.next()
                for k in range(8):
                    op("pe", lambda e: e.transpose(out=pt[:, k, :], in_=c_[:, k * 128:(k + 1) * 128], identity=identb[:]),
                       reads=[c_, identb], writes=[pt])
                cT = catT.next()
                op("act", lambda e: e.copy(out=cT[:], in_=pt[:]), reads=[pt], writes=[cT])
                yield
                x1_ = x1.next()
                for hf in range(2):
                    pm = pMx.next()
                    for k in range(8):
                        op("pe", lambda e: e.matmul(pm[:], lhsT=cT[:, k, :], rhs=wo[:, k, hf * 512:(hf + 1) * 512], start=(k == 0),
                                                    stop=(k == 7)), reads=[cT] + wo_all, writes=[pm])
                    hs = slice(hf * 512, (hf + 1) * 512)
                    op("dve", lambda e: e.tensor_tensor(out=x1_[:, hs], in0=pm[:], in1=g1bc[:, s, hs], op=ALU.mult),
                       reads=[pm, g1bc], writes=[x1_.sub(hf)])
                    op("pool", lambda e: e.tensor_tensor(out=x1_[:, hs], in0=x1_[:, hs], in1=x_[:, hs], op=ALU.add),
                       reads=[x1_.sub(hf), x_], writes=[x1_.sub(hf)])
                    yield
                x1a = [x1_.sub(0), x1_.sub(1)]
                Sx.dma("sp", x1_d[ti * 128:(ti + 1) * 128, :], x1_[:], reads=x1a, writes=[R_x1])
                st_ = st4.next()
                op("act", lambda e: e.activation(out=sq4[:], in_=x1_[:], func=AF.Square, accum_out=st_[:, 0:1]), reads=x1a, writes=[sq4, st_])
                yield
                op("dve", lambda e: e.tensor_scalar(out=st_[:, 0:1], in0=st_[:, 0:1], scalar1=1.0 / D, scalar2=EPS, op0=ALU.mult, op1=ALU.add),
                   reads=[st_], writes=[st_])
                op("pool", lambda e: e.tensor_tensor(out=st_[:, 1:2], in0=st_[:, 0:1], in1=mhalf[:, 0:1], op=ALU.pow), reads=[st_, mhalf], writes=[st_])
                yield
                h2_ = h2.next()
                h2b_ = h2b.next()
                op("dve", lambda e: e.scalar_tensor_tensor(out=h2_[:], in0=x1_[:], scalar=st_[:, 1:2], in1=A2bc[:, s, :], op0=ALU.mult, op1=ALU.mult),
                   reads=x1a + [st_, A2bc], writes=[h2_])
                yield
                op("pool", lambda e: e.tensor_tensor(out=h2_[:], in0=h2_[:], in1=s2bc[:, s, :], op=ALU.add), reads=[h2_, s2bc], writes=[h2_])
                yield
                op("act", lambda e: e.copy(out=h2b_[:], in_=h2_[:]), reads=[h2_], writes=[h2b_])
                Sx.dma("sp", h2_d[ti * 128:(ti + 1) * 128, :], h2b_[:], reads=[h2b_], writes=[R_h2])
                hT_ = h2T.next()
                for hf in range(2):
                    pr = pR.next()
                    for k in range(4):
                        kk = hf * 4 + k
                        op("pe", lambda e: e.transpose(out=pr[:, k, :], in_=h2_[:, kk * 128:(kk + 1) * 128], identity=identf[:]),
                           reads=[h2_, identf], writes=[pr])
                    if hf == 0:
                        op("act", lambda e: e.copy(out=hT_[:, 0:4, :], in_=pr[:]), reads=[pr], writes=[hT_.sub(0)])
                    else:
                        op("dve", lambda e: e.tensor_copy(out=hT_[:, 4:8, :], in_=pr[:]), reads=[pr], writes=[hT_.sub(1)])
                    yield
                pl = pL.next()
                for k in range(8):
                    op("pe", lambda e: e.matmul(pl[:, 0:NE], lhsT=hT_[:, k, :], rhs=wr[:, k, :], start=(k == 0), stop=(k == 7)),
                       reads=[hT_.sub(0), hT_.sub(1), wr], writes=[pl])
                lgt = lg_all.sub(ti)
                op("dve", lambda e: e.tensor_tensor(out=lg_all[:, ti, :], in0=pl[:, 0:NE], in1=brt[:], op=ALU.add), reads=[pl, brt], writes=[lgt])
                t8 = top8_all.sub(ti)
                op("dve", lambda e: e.max(out=top8_all[:, ti, :], in_=lg_all[:, ti, :]), reads=[lgt], writes=[t8])
                yield
                op("dve", lambda e: e.tensor_scalar(out=st_[:, 2:3], in0=top8_all[:, ti, 0:1], scalar1=-1.0, scalar2=None, op0=ALU.mult),
                   reads=[t8], writes=[st_])
                op("dve", lambda e: e.tensor_scalar(out=M_all[:, ti, :], in0=lg_all[:, ti, :], scalar1=top8_all[:, ti, 3:4], scalar2=None,
                                                    op0=ALU.is_ge), reads=[lgt, t8], writes=[M_all.sub(ti)])
                yield
                e4_ = e4.next()
                op("act", lambda e: e.activation(out=e4_[:], in_=top8_all[:, ti, 0:4], func=AF.Exp, bias=st_[:, 2:3], accum_out=st_[:, 3:4]),
                   reads=[t8, st_], writes=[e4_, st_])
                yield
                op("dve", lambda e: e.reciprocal(out=st_[:, 3:4], in_=st_[:, 3:4]), reads=[st_], writes=[st_])
                yield
                op("dve", lambda e: e.tensor_scalar(out=G4[:, ti, :], in0=e4_[:], scalar1=st_[:, 3:4], scalar2=None, op0=ALU.mult),
                   reads=[e4_, st_], writes=[G4.sub(ti)])

            interleave((tile4(ti) for ti in range(NT)), NW)
            Sx.barrier()
        if stop_after == 4:
            Sx.finish()
            return nc

        M_res = [M_all.sub(ti) for ti in range(NT)]
        with ExitStack() as ph:
            sb, ps = mk_alloc(ph, "p5")
            pC = ps("pC", [128, 512], F32)
            pP = Ring([ps("pP%d" % i, [128, 512], F32) for i in range(4)])
            cntf = sb("cntf", [128, NE], F32)
            cnti = sb("cnti", [128, NE], I32)
            padf = sb("padf", [128, NE], F32)
            pend = sb("pend", [128, NE], F32)
            pstart = sb("pstart", [128, NE], F32)
            onesf = sb("onesf", [128, NE], F32)
            junk = sb("junk", [128, NE], F32)
            blkf = sb("blkf", [128, NBLK], F32)
            pidf = sb("pidf", [128, 1], F32)
            pidi = sb("pidi", [128, 1], I32)
            tokid = sb("tokid", [128, NT], I32)
            zer = sb("zer", [128, NROWS // 128], I32)
            for ti in range(NT):
                op("pe", lambda e: e.matmul(pC[:, 0:NE], lhsT=onesb[:], rhs=M_all[:, ti, :], start=(ti == 0), stop=(ti == NT - 1)),
                   reads=[onesb, M_res[ti]], writes=[pC])
            op("dve", lambda e: e.tensor_scalar(out=cntf[:], in0=pC[:, 0:NE], scalar1=float(BLK - 1), scalar2=None, op0=ALU.add), reads=[pC], writes=[cntf])
            op("dve", lambda e: e.tensor_copy(out=cnti[:], in_=cntf[:]), reads=[cntf], writes=[cnti])
            op("dve", lambda e: e.tensor_scalar(out=cnti[:], in0=cnti[:], scalar1=LOGB, scalar2=LOGB, op0=ALU.arith_shift_right,
                                                op1=ALU.logical_shift_left), reads=[cnti], writes=[cnti])
            op("dve", lambda e: e.tensor_copy(out=padf[:], in_=cnti[:]), reads=[cnti], writes=[padf])
            op("dve", lambda e: e.memset(onesf[:], 1.0), writes=[onesf])
            op("dve", lambda e: e.tensor_tensor_scan(out=pend[:], data0=onesf[:], data1=padf[:], initial=0.0, op0=ALU.mult, op1=ALU.add),
               reads=[onesf, padf], writes=[pend])
            op("dve", lambda e: e.tensor_tensor(out=pstart[:], in0=pend[:], in1=padf[:], op=ALU.subtract), reads=[pend, padf], writes=[pstart])
            thri = sb("thri", [128, NBLK], I32)
            thrf = sb("thrf", [128, NBLK], F32)
            cmpb = sb("cmpb", [128, NBLK, NE], F32)
            op("pool", lambda e: e.iota(thri[:], pattern=[[BLK, NBLK]], base=0, channel_multiplier=0), writes=[thri])
            op("dve", lambda e: e.tensor_copy(out=thrf[:], in_=thri[:]), reads=[thri], writes=[thrf])
            op("dve", lambda e: e.tensor_tensor(out=cmpb[:], in0=pend[:].unsqueeze(1).to_broadcast([128, NBLK, NE]),
                                                in1=thrf[:].unsqueeze(2).to_broadcast([128, NBLK, NE]), op=ALU.is_le),
               reads=[pend, thrf], writes=[cmpb])
            op("dve", lambda e: e.tensor_reduce(out=blkf[:], in_=cmpb[:], axis=AX.X, op=ALU.add), reads=[cmpb], writes=[blkf])
            op("dve", lambda e: e.tensor_scalar(out=blkf[:], in0=blkf[:], scalar1=float(NE - 1), scalar2=None, op0=ALU.min), reads=[blkf], writes=[blkf])
            op("pool", lambda e: e.iota(pidi[:], pattern=[[0, 1]], base=0, channel_multiplier=1), writes=[pidi])
            op("dve", lambda e: e.tensor_copy(out=pidf[:], in_=pidi[:]), reads=[pidi], writes=[pidf])
            wtmp = sb("wtmp", [128, NBLK], F32)
            op("dve", lambda e: e.tensor_scalar(out=wtmp[:], in0=blkf[:], scalar1=128.0, scalar2=pidf[:, 0:1], op0=ALU.mult, op1=ALU.add),
               reads=[blkf, pidf], writes=[wtmp])
            op("dve", lambda e: e.tensor_copy(out=widx[:], in_=wtmp[:]), reads=[wtmp], writes=[widx])
            op("dve", lambda e: e.tensor_copy(out=eidx[:], in_=blkf[:]), reads=[blkf], writes=[eidx])
            op("pool", lambda e: e.iota(tokid[:], pattern=[[128, NT]], base=0, channel_multiplier=1), writes=[tokid])
            op("pool", lambda e: e.memset(zer[:], 0), writes=[zer])
            Sx.dma("sp", rtok_d.rearrange("(p a) o -> p (a o)", p=128), zer[:], reads=[zer], writes=[R_rtok])
            dd = Ring([sb("dd%d" % i, [128, NE], F32) for i in range(2)])
            oh = Ring([sb("oh%d" % i, [128, 4, NE], F32) for i in range(2)])
            destf = sb("destf", [128, NT, 4], F32)
            for ti in range(NT):
                pp = pP.next()
                op("pe", lambda e: e.matmul(pp[:, 0:NE], lhsT=ustr[:], rhs=M_all[:, ti, :], start=True, stop=(ti == 0)), reads=[ustr, M_res[ti]], writes=[pp])
                for t2 in range(ti):
                    op("pe", lambda e: e.matmul(pp[:, 0:NE], lhsT=onesb[:], rhs=M_all[:, t2, :], start=False, stop=(t2 == ti - 1)),
                       reads=[onesb, M_res[t2]], writes=[pp])
                d_ = dd.next()
                op("dve", lambda e: e.tensor_tensor(out=d_[:], in0=pp[:, 0:NE], in1=pstart[:], op=ALU.add), reads=[pp, pstart], writes=[d_])
                o_ = oh.next()
                op("dve", lambda e: e.tensor_tensor(out=o_[:], in0=lg_all[:, ti, :].unsqueeze(1).to_broadcast([128, 4, NE]),
                                                     in1=top8_all[:, ti, 0:4].unsqueeze(2).to_broadcast([128, 4, NE]), op=ALU.is_equal),
                   reads=[lg_all.sub(ti), top8_all.sub(ti)], writes=[o_])
                op("dve", lambda e: e.tensor_tensor(out=o_[:], in0=o_[:], in1=d_[:].unsqueeze(1).to_broadcast([128, 4, NE]), op=ALU.mult),
                   reads=[o_, d_], writes=[o_])
                op("dve", lambda e: e.tensor_reduce(out=destf[:, ti, :], in_=o_[:], axis=AX.X, op=ALU.add), reads=[o_], writes=[destf])
            op("dve", lambda e: e.tensor_copy(out=dest_i[:], in_=destf[:]), reads=[destf], writes=[dest_i])
            for ti in range(NT):
                for k in range(4):
                    Sx.idma(rtok_d, tokid[:, ti:ti + 1], dest_i[:, ti, k:k + 1], gather=False, reads=[tokid, dest_i], writes=[R_rtok], slot="scat", shared=True)
            Sx.barrier()
        if stop_after == 5:
            Sx.finish()
            return nc

        with ExitStack() as ph:
            sb, ps = mk_alloc(ph, "p6")
            wgu = Ring([sb("wgu%d" % i, [128, 8, 2048], BF16) for i in range(2)])
            wdn = Ring([sb("wdn%d" % i, [128, 8, D], BF16) for i in range(2)])
            bgu = Ring([sb("bgu%d" % i, [128, 16], F32) for i in range(2)])
            bdn = Ring([sb("bdn%d" % i, [128, D], F32) for i in range(2)])
            bgu1 = Ring([sb("bgu1_%d" % i, [128, 8], F32) for i in range(2)])
            idx = Ring([sb("idx%d" % i, [128, JB], I32) for i in range(2)])
            xb = Ring([sb("xb%d" % i, [128, JB, D], BF16) for i in range(2)])
            xbT = Ring([sb("xbT%d" % i, [128, 8, BLK], BF16) for i in range(1)])
            actT = Ring([sb("actT%d" % i, [128, 8, BLK], BF16) for i in range(1)])
            gt_ = Ring([sb("gt%d" % i, [128, BLK], F32) for i in range(2)])
            sg_ = Ring([sb("sg%d" % i, [128, BLK], F32) for i in range(2)])
            ut_ = Ring([sb("ut%d" % i, [128, BLK], F32) for i in range(1)])
            osb = Ring([sb("osb%d" % i, [128, 512], F32) for i in range(10)])
            pX = Ring([ps("pX%d" % i, [128, 8, 128], BF16) for i in range(2)])
            pGU = Ring([ps("pGU%d" % i, [128, 512], F32) for i in range(4)])
            pO = Ring([ps("pO%d" % i, [128, 512], F32) for i in range(2)])

            def load_a(b):
                wg_, bg_, ix_, xb_ = wgu.next(), bgu.next(), idx.next(), xb.next()
                Sx.dma("sp", ix_[:], rtok_d[b * BLK:(b + 1) * BLK, :].rearrange("(p j) o -> p (j o)", p=128), reads=[R_rtok], writes=[ix_])
                for j in range(JB):
                    Sx.idma(xb_[:, j, :], h2_d, ix_[:, j:j + 1], gather=True, reads=[ix_, R_h2], writes=[xb_.sub(j)])
                Sx.idma(wg_[:].rearrange("p k n -> p (k n)"), wgub_d, widx[:, b:b + 1], gather=True, reads=[widx], writes=[wg_])
                Sx.idma(bg_[:], b_gu_d, widx[:, b:b + 1], gather=True, reads=[widx], writes=[bg_])
                return wg_, bg_, xb_

            def load_b(b):
                wd_, bd_ = wdn.next(), bdn.next()
                Sx.idma(wd_[:].rearrange("p k n -> p (k n)"), wdnb_d, widx[:, b:b + 1], gather=True, reads=[widx], writes=[wd_])
                Sx.idma(bd_[:], b_dn_d, eidx[:, b:b + 1], gather=True, reads=[eidx], writes=[bd_])
                return wd_, bd_

            def do_T(xb_):
                xT = xbT.next()
                for j in range(JB):
                    px = pX.next()
                    for k in range(8):
                        op("pe", lambda e: e.transpose(out=px[:, k, :], in_=xb_[:, j, k * 128:(k + 1) * 128], identity=identb[:]),
                           reads=[xb_.sub(j), identb], writes=[px])
                    op("act", lambda e: e.copy(out=xT[:, :, j * 128:(j + 1) * 128], in_=px[:]), reads=[px], writes=[xT.sub(j)])
                return xT

            A = {0: load_a(0)}
            Bw = {0: load_b(0)}
            if NBLK > 1:
                A[1] = load_a(1)
                Bw[1] = load_b(1)
            xT = do_T(A[0][2])
            for b in range(NBLK):
                wg_, bg_, xb_ = A.pop(b)
                wd_, bd_ = Bw.pop(b)
                wg_all = [wg_]
                wd_all = [wd_]
                xT_all = [xT.sub(j) for j in range(JB)]
                aT = actT.next()
                bg1 = bgu1.next()
                op("dve", lambda e: e.tensor_scalar(out=bg1[:], in0=bg_[:, 8:16], scalar1=1.0, scalar2=None, op0=ALU.add), reads=[bg_], writes=[bg1])
                for fc in range(8):
                    pg = pGU.next()
                    for k in range(8):
                        op("pe", lambda e: e.matmul(pg[:, 0:BLK], lhsT=wg_[:, k, fc * 128:(fc + 1) * 128], rhs=xT[:, k, :], start=(k == 0), stop=(k == 7)),
                           reads=xT_all + wg_all, writes=[pg])
                    pu = pGU.next()
                    for k in range(8):
                        op("pe", lambda e: e.matmul(pu[:, 0:BLK], lhsT=wg_[:, k, 1024 + fc * 128:1024 + (fc + 1) * 128], rhs=xT[:, k, :], start=(k == 0),
                                                    stop=(k == 7)), reads=xT_all + wg_all, writes=[pu])
                    g_, s_, u_ = gt_.next(), sg_.next(), ut_.next()
                    op("dve", lambda e: e.tensor_scalar(out=g_[:], in0=pg[:, 0:BLK], scalar1=bg_[:, fc:fc + 1], scalar2=7.0, op0=ALU.add, op1=ALU.min),
                       reads=[pg, bg_], writes=[g_])
                    op("act", lambda e: e.activation(out=s_[:], in_=g_[:], func=AF.Sigmoid, scale=1.702), reads=[g_], writes=[s_])
                    op("dve", lambda e: e.tensor_scalar(out=u_[:], in0=pu[:, 0:BLK], scalar1=bg1[:, fc:fc + 1], scalar2=8.0, op0=ALU.add, op1=ALU.min),
                       reads=[pu, bg1], writes=[u_])
                    op("pool", lambda e: e.tensor_tensor(out=g_[:], in0=g_[:], in1=s_[:], op=ALU.mult), reads=[g_, s_], writes=[g_])
                    op("dve", lambda e: e.scalar_tensor_tensor(out=aT[:, fc, :], in0=u_[:], scalar=-6.0, in1=g_[:], op0=ALU.max, op1=ALU.mult),
                       reads=[g_, u_], writes=[aT.sub(fc)])
                aT_all = [aT.sub(fc) for fc in range(8)]
                if b + 2 < NBLK:
                    A[b + 2] = load_a(b + 2)
                if b + 1 < NBLK:
                    xT = do_T(A[b + 1][2])
                for j in range(JB):
                    for hf in range(2):
                        o_ = osb.next()
                        po = pO.next()
                        for fc in range(8):
                            op("pe", lambda e: e.matmul(po[:], lhsT=aT[:, fc, j * 128:(j + 1) * 128], rhs=wd_[:, fc, hf * 512:(hf + 1) * 512],
                                                        start=(fc == 0), stop=(fc == 7)), reads=aT_all + wd_all, writes=[po])
                        op("dve", lambda e: e.tensor_tensor(out=o_[:], in0=po[:], in1=bd_[:, hf * 512:(hf + 1) * 512], op=ALU.add),
                           reads=[po, bd_], writes=[o_])
                        Sx.dma("sp", oslot_d[b * BLK:(b + 1) * BLK, :].rearrange("(p j) d -> p j d", p=128)[:, j, hf * 512:(hf + 1) * 512], o_[:],
                               reads=[o_], writes=[R_oslot])
                if b + 2 < NBLK:
                    Bw[b + 2] = load_b(b + 2)
            Sx.barrier()
        if stop_after == 6:
            Sx.finish()
            return nc

        with ExitStack() as ph:
            sb, ps = mk_alloc(ph, "p7")
            g2bc = sb("g2bc", [128, NSEQ, D], F32)
            for s in range(NSEQ):
                Sx.dma("sp", g2bc[:, s, :], mod_d[s, 5 * D:6 * D].partition_broadcast(128), reads=[R_mod], writes=[g2bc])
            NW7 = 6
            rows = Ring([sb("rows%d" % i, [128, 4, D], F32) for i in range(NW7)])
            x1t = Ring([sb("x1t%d" % i, [128, D], F32) for i in range(NW7)])
            accr = Ring([sb("accr%d" % i, [128, D], F32) for i in range(NW7)])

            def tile7(ti):
                s = (ti * 128) // S
                r_, x_ = rows.next(), x1t.next()
                for k in range(4):
                    Sx.idma(r_[:, k, :], oslot_d, dest_i[:, ti, k:k + 1], gather=True, reads=[dest_i, R_oslot], writes=[r_.sub(k)],
                            slot="L_%s_%d" % (r_.r.name, k))
                Sx.dma("sp", x_[:], x1_d[ti * 128:(ti + 1) * 128, :], reads=[R_x1], writes=[x_])
                yield
                a_ = accr.next()
                op("dve", lambda e: e.tensor_scalar(out=a_[:], in0=r_[:, 0, :], scalar1=G4[:, ti, 0:1], scalar2=None, op0=ALU.mult),
                   reads=[r_.sub(0), G4.sub(ti)], writes=[a_])
                yield
                for k in range(1, 4):
                    op("dve", lambda e: e.scalar_tensor_tensor(out=a_[:], in0=r_[:, k, :], scalar=G4[:, ti, k:k + 1], in1=a_[:], op0=ALU.mult,
                                                               op1=ALU.add), reads=[r_.sub(k), G4.sub(ti), a_], writes=[a_])
                    yield
                op("pool", lambda e: e.tensor_tensor(out=a_[:], in0=a_[:], in1=g2bc[:, s, :], op=ALU.mult), reads=[a_, g2bc], writes=[a_])
                yield
                op("dve", lambda e: e.tensor_tensor(out=a_[:], in0=a_[:], in1=x_[:], op=ALU.add), reads=[a_, x_], writes=[a_])
                Sx.dma("sp", out_d[ti * 128:(ti + 1) * 128, :], a_[:], reads=[a_], writes=[])

            interleave((tile7(ti) for ti in range(NT)), NW7)
        Sx.finish()
        build_program.stats = (Sx.ninst, Sx.nwait, dict(usage))
    return nc


def _host_layout(inputs, S):
    f = lambda a: np.ascontiguousarray(np.asarray(a, dtype=np.float32))
    kp = lambda w: np.ascontiguousarray(w.reshape(8, 128, -1).transpose(1, 0, 2))
    L = {}
    L["w_ada_l"] = kp(f(inputs["w_ada"])[0])
    L["b_ada"] = f(inputs["b_ada"])
    L["norm1_g"] = f(inputs["norm1_g"])
    L["norm2_g"] = f(inputs["norm2_g"])
    L["w_in_l"] = kp(f(inputs["w_in"])[0])
    L["conv_w"] = f(inputs["conv_w"])[0]
    L["conv_b"] = f(inputs["conv_b"])
    for n in ("b_igate", "b_fgate", "mlstm_norm_g", "q_norm_g", "k_norm_g", "lambda_q1", "lambda_k1", "lambda_q2", "lambda_k2",
              "diff_norm_g", "b_router"):
        L[n] = f(inputs[n])
    L["w_out_l"] = kp(f(inputs["w_out"])[0])
    L["w_router_l"] = kp(f(inputs["w_router"])[0])
    wgu = f(inputs["w_gu"])[0]
    wgu = wgu.reshape(NE, 8, 128, 1024, 2).transpose(0, 2, 1, 4, 3)
    L["w_gu_l"] = np.ascontiguousarray(wgu).reshape(NE * 128, 16384)
    bgu = f(inputs["b_gu"])[0].reshape(NE, 8, 128, 2).transpose(0, 2, 3, 1)
    L["b_gu_l"] = np.ascontiguousarray(bgu).reshape(NE * 128, 16)
    wdn = f(inputs["w_down"])[0].reshape(NE, 8, 128, D).transpose(0, 2, 1, 3)
    L["w_down_l"] = np.ascontiguousarray(wdn).reshape(NE * 128, 8192)
    L["b_down"] = f(inputs["b_down"])[0]
    pos = np.arange(S, dtype=np.float32)
    inv_freq = (np.float32(10000.0) ** (-np.arange(0, 64, 2, dtype=np.float32) / np.float32(64))).astype(np.float32)
    ang = (pos[:, None] * inv_freq[None, :]).astype(np.float32)
    L["cos_t"] = np.cos(ang).astype(np.float32)
    L["sin_t"] = np.sin(ang).astype(np.float32)
    return L


def run(inputs, n_cores, BLK=512, stop_after=None, debug=False):
    x = np.asarray(inputs["x"], dtype=np.float32)
    c = np.asarray(inputs["c"], dtype=np.float32)
    B, S, _ = x.shape
    NSEQ = B // n_cores
    L = _host_layout(inputs, S)
    nc = build_program(NSEQ, S, BLK, stop_after=stop_after, debug=debug)
    in_maps = []
    for i in range(n_cores):
        m = dict(L)
        m["x"] = np.ascontiguousarray(x[i * NSEQ:(i + 1) * NSEQ].reshape(NSEQ * S, D))
        m["c"] = np.ascontiguousarray(c[i * NSEQ:(i + 1) * NSEQ])
        in_maps.append(m)
    res = run_bass_kernel_spmd(nc, in_maps, core_ids=list(range(n_cores)))
    out = np.concatenate([r["out"].reshape(NSEQ, S, D) for r in res.results], axis=0)
    return out, res


def kernel(**inputs):
    out, _ = run(inputs, N_CORES, BLK=512)
    return out.astype(np.float32)
```
